# Optimizing a Trainium2 kernel written in Bass

```python
import math
import jax, jax.numpy as jnp
from jax import lax
import numpy as np

D_MODEL = 1024
BATCH = 4
SEQ = 4096
DEPTH = 2

N_MIXERS = 2
DA_HEADS = 8
DA_HEAD_DIM = D_MODEL // DA_HEADS // 2
DA_V_DIM = 2 * DA_HEAD_DIM
ROPE_THETA = 10000.0
Q_BLOCK = 128
CONV_WIDTH = 31
D_FF_DENSE = 2816
N_EXPERTS = 8
TOP_K = 2
D_FF_EXPERT = 3584
MOE_BLOCK = 128
EPS = 1e-6
N_EVEN = (DEPTH + 1) // 2
N_ODD = DEPTH // 2

kernel_name = "hybrid_diffattn_conformer_moe_adaln"


def rms_norm(x, g):
    xf = x.astype(jnp.float32)
    y = xf * lax.rsqrt(jnp.mean(xf * xf, axis=-1, keepdims=True) + EPS)
    return (y * g.astype(jnp.float32)).astype(x.dtype)


def layer_norm(x, g, b):
    xf = x.astype(jnp.float32)
    mu = jnp.mean(xf, axis=-1, keepdims=True)
    var = jnp.mean(jnp.square(xf - mu), axis=-1, keepdims=True)
    y = (xf - mu) * lax.rsqrt(var + EPS)
    return (y * g.astype(jnp.float32) + b.astype(jnp.float32)).astype(x.dtype)


def modulate(h, shift, scale):
    return h * (1 + scale[:, None, :]) + shift[:, None, :]


def rope(x, positions):
    dh = x.shape[-1]
    half = dh // 2
    inv_freq = ROPE_THETA ** (-jnp.arange(half, dtype=jnp.float32) / half)
    ang = positions.astype(jnp.float32)[..., None] * inv_freq
    cos = jnp.cos(ang)[:, :, None, :]
    sin = jnp.sin(ang)[:, :, None, :]
    xf = x.astype(jnp.float32)
    x1, x2 = xf[..., :half], xf[..., half:]
    out = jnp.concatenate([x1 * cos - x2 * sin, x2 * cos + x1 * sin], axis=-1)
    return out.astype(x.dtype)


def diff_attention(h, positions, w_qkv, w_o, lam_q1, lam_k1, lam_q2, lam_k2,
                   subln_g, lambda_init):
    B, T, D = h.shape
    qkv = h @ w_qkv
    q, k, v = jnp.split(qkv, 3, axis=-1)
    q = q.reshape(B, T, 2 * DA_HEADS, DA_HEAD_DIM)
    k = k.reshape(B, T, 2 * DA_HEADS, DA_HEAD_DIM)
    v = v.reshape(B, T, DA_HEADS, DA_V_DIM)
    q = rope(q, positions) * (DA_HEAD_DIM ** -0.5)
    k = rope(k, positions)
    lam = (jnp.exp(jnp.sum(lam_q1.astype(jnp.float32) * lam_k1.astype(jnp.float32)))
           - jnp.exp(jnp.sum(lam_q2.astype(jnp.float32) * lam_k2.astype(jnp.float32)))
           + lambda_init)
    nb = T // Q_BLOCK
    qb = q.reshape(B, nb, Q_BLOCK, 2 * DA_HEADS, DA_HEAD_DIM).transpose(1, 0, 3, 2, 4)
    kt = k.transpose(0, 2, 1, 3)
    vt = v.transpose(0, 2, 1, 3)
    key_pos = jnp.arange(T)

    def block(args):
        q_blk, start = args
        s = jnp.einsum('bhqd,bhkd->bhqk', q_blk, kt).astype(jnp.float32)
        q_pos = start + jnp.arange(Q_BLOCK)
        causal = key_pos[None, :] <= q_pos[:, None]
        s = jnp.where(causal, s, -jnp.inf)
        p = jax.nn.softmax(s, axis=-1).reshape(B, DA_HEADS, 2, Q_BLOCK, T)
        a = p[:, :, 0] - lam * p[:, :, 1]
        return jnp.einsum('bhqk,bhkd->bhqd', a.astype(vt.dtype), vt)

    o = lax.map(block, (qb, jnp.arange(nb) * Q_BLOCK))
    o = o.transpose(1, 0, 3, 2, 4).reshape(B, T, DA_HEADS, DA_V_DIM)
    o = rms_norm(o, subln_g) * (1 - lambda_init)
    return o.reshape(B, T, DA_HEADS * DA_V_DIM) @ w_o


def conformer_conv(h, w_pw1, b_pw1, w_dw, b_dw, ln_g, ln_b, w_pw2, b_pw2):
    D = h.shape[-1]
    u = h @ w_pw1 + b_pw1
    a, g = jnp.split(u, 2, axis=-1)
    u = a * jax.nn.sigmoid(g)
    u = lax.conv_general_dilated(
        u, w_dw[:, None, :], window_strides=(1,),
        padding=[(CONV_WIDTH - 1, 0)],
        dimension_numbers=('NWC', 'WIO', 'NWC'),
        feature_group_count=D) + b_dw
    u = jax.nn.silu(layer_norm(u, ln_g, ln_b))
    return u @ w_pw2 + b_pw2


def dense_swiglu(h, w_gate, w_up, w_down):
    return (jax.nn.silu(h @ w_gate) * (h @ w_up)) @ w_down


def moe_swiglu(h, w_router, w_gate, w_up, w_down):
    B, T, D = h.shape
    xt = h.reshape(-1, D)
    N = xt.shape[0]
    A = N * TOP_K
    logits = (xt @ w_router).astype(jnp.float32)
    top_val, top_idx = lax.top_k(logits, TOP_K)
    gates = jax.nn.softmax(top_val, axis=-1)
    flat_e = top_idx.reshape(-1)
    order = jnp.argsort(flat_e)
    tok = order // TOP_K
    sorted_e = flat_e[order]
    sizes = jnp.bincount(flat_e, length=N_EXPERTS)
    padded = (sizes + MOE_BLOCK - 1) // MOE_BLOCK * MOE_BLOCK
    pad_end = jnp.cumsum(padded)
    pad_start = pad_end - padded
    grp_start = jnp.cumsum(sizes) - sizes
    dest = pad_start[sorted_e] + (jnp.arange(A) - grp_start[sorted_e])
    P = A + N_EXPERTS * MOE_BLOCK
    nb = P // MOE_BLOCK
    block_e = jnp.minimum(
        jnp.searchsorted(pad_end, jnp.arange(nb) * MOE_BLOCK, side='right'),
        N_EXPERTS - 1)
    x_pad = jnp.zeros((P, D), xt.dtype).at[dest].set(xt[tok])

    def expert_block(args):
        xb, e = args
        return (jax.nn.silu(xb @ w_gate[e]) * (xb @ w_up[e])) @ w_down[e]

    y_pad = lax.map(expert_block, (x_pad.reshape(nb, MOE_BLOCK, D), block_e)).reshape(P, D)
    g_sorted = gates.reshape(-1)[order].astype(xt.dtype)
    y = y_pad[dest] * g_sorted[:, None]
    out = jnp.zeros_like(xt).at[tok].add(y)
    return out.reshape(B, T, D)


def setup_inputs(seed: int = 0) -> dict:
    key = jax.random.key(seed)
    ks = iter(jax.random.split(key, 40))
    D = D_MODEL
    f32 = jnp.float32

    def nrm(shape, fan_in, mult=1.0):
        return jax.random.normal(next(ks), shape, f32) * (mult * fan_in ** -0.5)

    def gain(shape):
        return 1.0 + 0.01 * jax.random.normal(next(ks), shape, f32)

    def bias(shape):
        return 0.01 * jax.random.normal(next(ks), shape, f32)

    x = jax.random.normal(next(ks), (BATCH, SEQ, D), f32)
    c = jax.random.normal(next(ks), (BATCH, D), f32)
    offset = jax.random.randint(next(ks), (BATCH, 1), 0, SEQ, dtype=jnp.int32)
    positions = jnp.arange(SEQ, dtype=jnp.int32)[None, :] + offset
    return {
        "x": x,
        "c": c,
        "positions": positions,
        "w_ada": nrm((DEPTH, D, 6 * D), D, 0.5),
        "b_ada": bias((DEPTH, 6 * D)),
        "norm_mix_g": gain((DEPTH, D)),
        "norm_ffn_g": gain((DEPTH, D)),
        "attn_w_qkv": nrm((N_EVEN, D, 3 * D), D),
        "attn_w_o": nrm((N_EVEN, D, D), D),
        "lam_q1": 0.1 * jax.random.normal(next(ks), (N_EVEN, DA_HEAD_DIM), f32),
        "lam_k1": 0.1 * jax.random.normal(next(ks), (N_EVEN, DA_HEAD_DIM), f32),
        "lam_q2": 0.1 * jax.random.normal(next(ks), (N_EVEN, DA_HEAD_DIM), f32),
        "lam_k2": 0.1 * jax.random.normal(next(ks), (N_EVEN, DA_HEAD_DIM), f32),
        "attn_subln_g": gain((N_EVEN, DA_V_DIM)),
        "conv_w_pw1": nrm((N_ODD, D, 2 * D), D),
        "conv_b_pw1": bias((N_ODD, 2 * D)),
        "conv_w_dw": nrm((N_ODD, CONV_WIDTH, D), CONV_WIDTH),
        "conv_b_dw": bias((N_ODD, D)),
        "conv_ln_g": gain((N_ODD, D)),
        "conv_ln_b": bias((N_ODD, D)),
        "conv_w_pw2": nrm((N_ODD, D, D), D),
        "conv_b_pw2": bias((N_ODD, D)),
        "ffn_w_gate": nrm((N_EVEN, D, D_FF_DENSE), D),
        "ffn_w_up": nrm((N_EVEN, D, D_FF_DENSE), D),
        "ffn_w_down": nrm((N_EVEN, D_FF_DENSE, D), D_FF_DENSE),
        "moe_w_router": nrm((N_ODD, D, N_EXPERTS), D),
        "moe_w_gate": nrm((N_ODD, N_EXPERTS, D, D_FF_EXPERT), D),
        "moe_w_up": nrm((N_ODD, N_EXPERTS, D, D_FF_EXPERT), D),
        "moe_w_down": nrm((N_ODD, N_EXPERTS, D_FF_EXPERT, D), D_FF_EXPERT),
        "final_g": gain((D,)),
    }


def reference(x, c, positions, w_ada, b_ada, norm_mix_g, norm_ffn_g,
              attn_w_qkv, attn_w_o, lam_q1, lam_k1, lam_q2, lam_k2, attn_subln_g,
              conv_w_pw1, conv_b_pw1, conv_w_dw, conv_b_dw, conv_ln_g, conv_ln_b,
              conv_w_pw2, conv_b_pw2, ffn_w_gate, ffn_w_up, ffn_w_down,
              moe_w_router, moe_w_gate, moe_w_up, moe_w_down, final_g):
    c_act = jax.nn.silu(c)
    for i in range(DEPTH):
        j = i // 2
        mod = c_act @ w_ada[i] + b_ada[i]
        sh_m, sc_m, g_m, sh_f, sc_f, g_f = jnp.split(mod, 6, axis=-1)
        h = modulate(rms_norm(x, norm_mix_g[i]), sh_m, sc_m)
        if i % N_MIXERS == 0:
            lambda_init = 0.8 - 0.6 * math.exp(-0.3 * i)
            mix = diff_attention(h, positions, attn_w_qkv[j], attn_w_o[j],
                                 lam_q1[j], lam_k1[j], lam_q2[j], lam_k2[j],
                                 attn_subln_g[j], lambda_init)
        else:
            mix = conformer_conv(h, conv_w_pw1[j], conv_b_pw1[j], conv_w_dw[j],
                                 conv_b_dw[j], conv_ln_g[j], conv_ln_b[j],
                                 conv_w_pw2[j], conv_b_pw2[j])
        x = x + g_m[:, None, :] * mix
        h = modulate(rms_norm(x, norm_ffn_g[i]), sh_f, sc_f)
        if i % 2 == 0:
            ff = dense_swiglu(h, ffn_w_gate[j], ffn_w_up[j], ffn_w_down[j])
        else:
            ff = moe_swiglu(h, moe_w_router[j], moe_w_gate[j], moe_w_up[j], moe_w_down[j])
        x = x + g_f[:, None, :] * ff
    return rms_norm(x, final_g)
```

```python
from contextlib import ExitStack
import math
import numpy as np
import ml_dtypes
import concourse.bass as bass
import concourse.mybir as mybir
from concourse.bass_utils import run_bass_kernel_spmd

F32 = mybir.dt.float32
BF16 = mybir.dt.bfloat16
I32 = mybir.dt.int32
AF = mybir.ActivationFunctionType
ALU = mybir.AluOpType
AX = mybir.AxisListType

SEM_LIM = 4000
D = 1024
NO = 2048
HALO = 32
NT = NO + HALO
NP = 2048
KC = 8
FF0 = 2816
FFE = 3584
NE = 8
EPS = 1e-6
NEG = -30000.0
CH = [(0, 512), (512, 512), (1024, 512), (1536, 512), (2048, 32)]
CHO = CH[:4]
LAMBDA_INIT0 = 0.8 - 0.6 * math.exp(-0.3 * 0)


import types


def freeze(fn, depth=0):
    if fn is None or not isinstance(fn, types.FunctionType) or fn.__closure__ is None or depth > 4:
        return fn
    cells = []
    for c in fn.__closure__:
        try:
            v = c.cell_contents
        except ValueError:
            cells.append(c)
            continue
        if isinstance(v, types.FunctionType):
            v = freeze(v, depth + 1)
        elif isinstance(v, tuple) and any(isinstance(x, types.FunctionType) for x in v):
            v = tuple(freeze(x, depth + 1) for x in v)
        cells.append(types.CellType(v))
    g = types.FunctionType(fn.__code__, fn.__globals__, fn.__name__, fn.__defaults__, tuple(cells))
    g.__kwdefaults__ = fn.__kwdefaults__
    return g


class T:
    __slots__ = ("name", "w", "r", "dsem", "dcount")

    def __init__(self, name=""):
        self.name = name
        self.w = None
        self.r = {}
        self.dsem = None
        self.dcount = 0


def TL(n, name=""):
    return [T(f"{name}{i}") for i in range(n)]


class Sched:
    ENGS = ("pe", "act", "dve", "pool", "sp")

    def __init__(self, nc, stack):
        self.nc = nc
        self.stack = stack
        self.ops = {e: [] for e in self.ENGS}
        self.count = {e: 0 for e in self.ENGS}
        self.esems = {e: [] for e in self.ENGS}
        self.seen = {e: {} for e in self.ENGS}
        self.nsem = 0
        self.final_waits = []
        self.dsems = []

    def new_sem(self, name):
        self.nsem += 1
        return self.stack.enter_context(self.nc.semaphore(f"{name}_{self.nsem}"))

    def _eng_sem(self, e, n):
        k = (n - 1) // SEM_LIM
        while len(self.esems[e]) <= k:
            self.esems[e].append(self.new_sem(f"p_{e}"))
        return self.esems[e][k], ((n - 1) % SEM_LIM) + 1

    def _deps(self, e, reads, writes):
        deps = {}

        def add(s, v):
            if deps.get(s, 0) < v:
                deps[s] = v
        for t in reads:
            if t.w is not None:
                add(*t.w)
        for t in writes:
            if t.w is not None:
                add(*t.w)
            for s, v in t.r.items():
                add(s, v)
        out = []
        own = self.esems[e]
        for s, v in deps.items():
            if e == "pe" and any(s is o for o in own):
                continue
            if self.seen[e].get(s, 0) >= v:
                continue
            self.seen[e][s] = v
            out.append((s, v))
        return out

    def _mark(self, sv, reads, writes):
        for t in reads:
            if t.r.get(sv[0], 0) < sv[1]:
                t.r[sv[0]] = sv[1]
        for t in writes:
            t.w = sv
            t.r = {}

    def op(self, e, fn, reads=(), writes=()):
        waits = self._deps(e, reads, writes)
        self.count[e] += 1
        sv = self._eng_sem(e, self.count[e])
        self.ops[e].append((waits, freeze(fn), (sv[0], 1)))
        self._mark(sv, reads, writes)
        return sv

    def dma(self, q, fn, reads=(), writes=(), sem_t=None):
        waits = self._deps(q, reads, writes)
        if sem_t is None:
            sem_t = writes[0] if writes else reads[0]
        if sem_t.dsem is None:
            sem_t.dsem = self.new_sem("d")
            self.dsems.append(sem_t)
        sem_t.dcount += 16
        sv = (sem_t.dsem, sem_t.dcount)
        self.ops[q].append((waits, freeze(fn), (sv[0], 16)))
        self._mark(sv, reads, writes)
        return sv

    def emit(self):
        nc = self.nc
        ops = self.ops
        finals = self.final_waits

        def run(eng, lst, extra=()):
            for waits, fn, inc in lst:
                for s, v in waits:
                    eng.wait_ge(s, v)
                if fn is None:
                    continue
                ins = fn(eng)
                ins.then_inc(inc[0], inc[1])
            for s, v in extra:
                eng.wait_ge(s, v)

        with nc.Block() as block:
            @block.tensor
            def _(eng):
                run(eng, ops["pe"])

            @block.scalar
            def _(eng):
                run(eng, ops["act"])

            @block.vector
            def _(eng):
                run(eng, ops["dve"])

            @block.gpsimd
            def _(eng):
                run(eng, ops["pool"])

            @block.sync
            def _(eng):
                run(eng, ops["sp"], finals)


W_SPECS = [
    ("w_ada", [2, D, 6 * D]), ("attn_w_qkv", [D, 3 * D]), ("attn_w_o", [D, D]),
    ("ffn_w_gate", [D, FF0]), ("ffn_w_up", [D, FF0]), ("ffn_w_down", [FF0, D]),
    ("conv_w_pw1", [D, 2 * D]), ("conv_w_pw2", [D, D]),
    ("moe_w_router", [D, NE]), ("moe_w_gate", [NE, D, FFE]), ("moe_w_up", [NE, D, FFE]),
    ("moe_w_down", [NE, FFE, D]),
]
S_SPECS = [
    ("xT_all", [D, NT], F32), ("xT_prev", [D, NP], F32),
    ("pos_kv", [1, NP + NO], I32),
    ("cT", [128, KC], F32), ("badaT", [128, 2, 48], F32),
    ("gmixT", [128, 2, KC], F32), ("gffnT", [128, 2, KC], F32), ("gfinT", [128, KC], F32),
    ("lamv", [128, 4, 64], F32), ("gsub", [128, 1], F32),
    ("bpw1T", [128, 16], F32), ("wdwT", [128, KC, 31], F32), ("bdwT", [128, KC], F32),
    ("lngT", [128, KC], F32), ("lnbT", [128, KC], F32), ("bpw2T", [128, KC], F32),
    ("invf", [128, 1], F32), ("visb", [128, 1], F32), ("hflag", [128, 1], F32),
    ("ident_b", [128, 128], BF16), ("ident_f", [128, 128], F32),
    ("masks", [128, 4, 512], BF16), ("maskh", [128, HALO], BF16), ("sel", [8, NE, 128], F32),
]


def build_program(stage=99):
    nc = bass.Bass("TRN2", target_bir_lowering=False)
    dr = {}
    for n, sh in W_SPECS:
        dr[n] = nc.dram_tensor(n, sh, F32, kind="ExternalInput").ap()
    for n, sh, dt in S_SPECS:
        dr[n] = nc.dram_tensor(n, sh, dt, kind="ExternalInput").ap()
    outT = nc.dram_tensor("outT", [D, NO], F32, kind="ExternalOutput").ap()

    with ExitStack() as st:
        S = Sched(nc, st)

        def sb(name, shape, dt):
            return st.enter_context(nc.sbuf_tensor(name, shape, dt))

        small = {}
        tsm = {}
        for n, sh, dt in S_SPECS:
            if n in ("xT_all", "xT_prev", "pos_kv"):
                continue
            small[n] = sb("s_" + n, sh, dt)
            tsm[n] = T(n)
            S.dma("sp", lambda e, n=n: e.dma_start(out=small[n][:], in_=dr[n]), writes=[tsm[n]])
        onesD = sb("onesD", [128, 128], BF16)
        t_ones = T("ones")
        S.op("pool", lambda e: e.memset(onesD[:], 1.0 / D), writes=[t_ones])
        epsc = sb("epsc", [128, 1], F32)
        t_epsc = T("epsc")
        S.op("pool", lambda e: e.memset(epsc[:], EPS), writes=[t_epsc])
        onesF = sb("onesF", [128, 128], F32)
        S.op("pool", lambda e: e.memset(onesF[:], 1.0 / D), writes=[t_ones])
        modv = sb("modv", [128, 2, 48], F32)
        t_mod = T("mod")
        Amix = sb("Amix", [128, 2, KC], F32)
        Affn = sb("Affn", [128, 2, KC], F32)
        t_A = T("A")
        cTb = sb("cTb", [128, KC], BF16)
        t_cTb = T("cTb")
        neglam = sb("neglam", [128, 1], F32)
        t_lam = T("lam")
        gsubc = sb("gsubc", [128, 1], F32)
        t_gsub = T("gsubc")
        onesF1 = sb("onesF1", [128, 128], F32)
        ones128 = sb("ones128", [128, 128], BF16)
        S.op("pool", lambda e: e.memset(onesF1[:], 1.0), writes=[t_ones])
        S.op("pool", lambda e: e.memset(ones128[:], 1.0 / 128), writes=[t_ones])

        ARENA_W = 48704
        arena = sb("arena", [128, ARENA_W], F32)
        psum = st.enter_context(nc.psum_tensor("psum", [128, 8, 512], F32))
        tP = TL(8, "ps")

        def view(off, nbytes, dt):
            assert off % 4 == 0 and nbytes % 4 == 0 and off + nbytes <= ARENA_W * 4, (off, nbytes)
            v = arena[:, off // 4:(off + nbytes) // 4]
            return v if dt == F32 else v.bitcast(dt)

        def barrier():
            svs = []
            for e in S.ENGS:
                if S.count[e] > 0:
                    svs.append(S._eng_sem(e, S.count[e]))
            for t in S.dsems:
                svs.append((t.dsem, t.dcount))
            bt = T("barrier")
            for e in S.ENGS:
                waits = []
                for s, v in svs:
                    if e == "pe" and any(s is o for o in S.esems[e]):
                        continue
                    if S.seen[e].get(s, 0) >= v:
                        continue
                    S.seen[e][s] = v
                    waits.append((s, v))
                if waits:
                    S.ops[e].append((waits, None, None))

        def mm_group(out_ap, pairs, reads, writes):
            def fn(e):
                n = len(pairs)
                ins = None
                for i, (l, r) in enumerate(pairs):
                    ins = e.matmul(out_ap, lhsT=l, rhs=r, start=(i == 0), stop=(i == n - 1))
                return ins
            return S.op("pe", fn, reads=reads, writes=writes)

        WA_OFF = 0
        wa = [view(WA_OFF + s * 8192, 8192, BF16).rearrange("p (k n) -> p k n", k=KC) for s in range(2)]
        t_wa = TL(2, "wa")
        S.op("act", lambda e: e.activation(out=cTb[:], in_=small["cT"][:], func=AF.Silu),
             reads=[tsm["cT"]], writes=[t_cTb])
        ada_wv = [dr["w_ada"][i].rearrange("(k p) n -> p k n", p=128) for i in range(2)]

        def ada_dma(i, pc, slot_ap, t_slot, extra_w=()):
            S.dma("pool", lambda e: e.dma_start(out=slot_ap, in_=ada_wv[i][:, :, pc * 512:(pc + 1) * 512]), writes=[t_slot] + list(extra_w))

        def ada_mm(i, pc, slot_ap, t_slot, bank):
            def fn(e):
                ins = None
                for jj in range(4):
                    for k in range(KC):
                        ins = e.matmul(psum[:, bank, jj:jj + 1], lhsT=slot_ap[:, k, jj * 128:(jj + 1) * 128],
                                       rhs=cTb[:, k:k + 1], start=(k == 0), stop=(k == KC - 1))
                return ins
            S.op("pe", fn, reads=[t_slot, t_cTb], writes=[tP[bank]])
            S.op("dve", lambda e: e.tensor_tensor(out=modv[:, i, pc * 4:pc * 4 + 4], in0=psum[:, bank, 0:4],
                                                  in1=small["badaT"][:, i, pc * 4:pc * 4 + 4], op=ALU.add),
                 reads=[tP[bank], tsm["badaT"]], writes=[t_mod])

        for pc in range(4):
            ada_dma(0, pc, wa[pc % 2], t_wa[pc % 2])
            ada_mm(0, pc, wa[pc % 2], t_wa[pc % 2], pc % 2)
        S.op("dve", lambda e: e.scalar_tensor_tensor(out=Amix[:, 0, :], in0=modv[:, 0, 8:16], scalar=1.0, in1=small["gmixT"][:, 0, :],
                                                     op0=ALU.add, op1=ALU.mult), reads=[t_mod, tsm["gmixT"]], writes=[t_A])
        ada_rest = [(0, pc) for pc in range(4, 12)] + [(1, pc) for pc in range(12)]
        lamt = sb("lamt", [128, 2, 64], F32)
        lams = sb("lams", [128, 2], F32)
        lame = sb("lame", [128, 2], F32)
        t_lt = T("lamt")
        S.op("dve", lambda e: e.tensor_tensor(out=lamt[:, 0, :], in0=small["lamv"][:, 0, :], in1=small["lamv"][:, 1, :], op=ALU.mult),
             reads=[tsm["lamv"]], writes=[t_lt])
        S.op("dve", lambda e: e.tensor_tensor(out=lamt[:, 1, :], in0=small["lamv"][:, 2, :], in1=small["lamv"][:, 3, :], op=ALU.mult),
             reads=[tsm["lamv"]], writes=[t_lt])
        S.op("dve", lambda e: e.tensor_reduce(out=lams[:], in_=lamt[:], axis=AX.X, op=ALU.add), reads=[t_lt], writes=[t_lt])
        S.op("act", lambda e: e.activation(out=lame[:], in_=lams[:], func=AF.Exp), reads=[t_lt], writes=[t_lt])
        S.op("dve", lambda e: e.scalar_tensor_tensor(out=neglam[:], in0=lame[:, 1:2], scalar=-LAMBDA_INIT0, in1=lame[:, 0:1],
                                                     op0=ALU.add, op1=ALU.subtract), reads=[t_lt], writes=[t_lam])
        S.op("dve", lambda e: e.tensor_scalar(out=gsubc[:], in0=small["gsub"][:], scalar1=1.0 - LAMBDA_INIT0, scalar2=None, op0=ALU.mult),
             reads=[tsm["gsub"]], writes=[t_gsub])

        R_X, R_H, R_S = 0, 66560, 99840
        xT = view(R_X, 66560, F32).rearrange("p (k n) -> p k n", k=KC)
        t_x = [TL(KC, f"x{ci}_") for ci in range(5)]
        hT = view(R_H, 33280, BF16).rearrange("p (k n) -> p k n", k=KC)
        t_h = TL(5, "h")
        hTp = view(R_X, 32768, BF16).rearrange("p (k n) -> p k n", k=KC)
        t_hp = TL(4, "hp")
        cosT = view(R_X + 32768, 8192, BF16)
        sinT = view(R_X + 40960, 8192, BF16)
        t_cs = T("cossin")
        wq5 = view(R_X + 49152, 5 * 2048, BF16).rearrange("p (w k n) -> p w k n", w=5, k=KC)
        t_w5 = TL(5, "w5")
        eO = [view(R_X + 49152 + 10240 + i * 2048, 2048, F32) for i in range(2)]
        t_eO = TL(2, "eO")
        asq = view(R_X + 49152 + 14336, 1024, BF16)
        t_asq = T("asq")
        ers = view(R_X + 49152 + 15360, 2048, F32)
        t_ers = T("ers")
        wo_sb = view(R_S + 30720, 16384, BF16).rearrange("p (k n) -> p k n", k=KC)
        t_wo = T("wo")
        o = R_S
        xs = view(o, 16384, F32).rearrange("p (k n) -> p k n", k=KC); o += 16384
        sqb = view(o, 8192, BF16).rearrange("p (k n) -> p k n", k=KC); o += 8192
        rstd = view(o, 2048, F32); o += 2048
        tmpA = view(o, 2048, F32); o += 2048
        tmpB = view(o, 2048, F32); o += 2048
        t_xs, t_sqb, t_rstd, t_tmpA, t_tmpB = T("xs"), T("sqb"), T("rstd"), T("tmpA"), T("tmpB")
        qz = view(o, 2 * NT * 2, BF16).rearrange("p (j n) -> p j n", j=2); o += 2 * NT * 2
        kT = view(o, 8192, BF16); o += 8192
        Vc = view(o, 32 * 130 * 2, BF16).rearrange("p (t n) -> p t n", t=32); o += 32 * 130 * 2
        t_q = TL(5, "q"); t_k = TL(8, "k"); t_v = TL(8, "v")
        NPB = 4
        pT = [view(o + i * 1024, 1024, BF16) for i in range(NPB)]; o += NPB * 1024
        t_pT = TL(NPB, "pT")
        oT = view(o, 33280, BF16).rearrange("p (k n) -> p k n", k=KC); o += 33280
        t_oT = TL(5, "oT")
        eL = [view(o, 2048, F32), rstd]; o += 2048
        t_eL = [T("eL0"), t_rstd]
        assert o <= ARENA_W * 4, o

        pos_t = view(R_S + 32768, 16384, I32)
        t_pos = T("pos")

        S.dma("sp", lambda e: e.dma_start(out=pos_t, in_=dr["pos_kv"][0:1, :].to_broadcast([128, NP + NO])), writes=[t_pos])
        TWO_PI = float(2 * np.pi)
        ang = view(R_S, 16384, F32)
        tq = view(R_H, 16384, F32)
        tqi = view(R_H, 16384, I32)
        t_ang, t_tq = T("ang"), T("tq")
        S.op("dve", lambda e: e.tensor_copy(out=tq, in_=pos_t), reads=[t_pos], writes=[t_tq])
        S.op("dve", lambda e: e.tensor_scalar(out=ang, in0=tq, scalar1=small["invf"][:, 0:1], scalar2=None, op0=ALU.mult),
             reads=[t_tq, tsm["invf"]], writes=[t_ang])
        S.op("dve", lambda e: e.tensor_scalar(out=tq, in0=ang, scalar1=1.0 / TWO_PI, scalar2=None, op0=ALU.mult),
             reads=[t_ang], writes=[t_tq])
        S.op("dve", lambda e: e.tensor_copy(out=tqi, in_=tq), reads=[t_tq], writes=[t_tq])
        S.op("dve", lambda e: e.tensor_copy(out=tq, in_=tqi), reads=[t_tq], writes=[t_tq])
        S.op("dve", lambda e: e.scalar_tensor_tensor(out=ang, in0=tq, scalar=-TWO_PI, in1=ang, op0=ALU.mult, op1=ALU.add),
             reads=[t_tq, t_ang], writes=[t_ang])
        S.op("dve", lambda e: e.tensor_scalar(out=tq, in0=ang, scalar1=float(np.pi), scalar2=None, op0=ALU.is_gt), reads=[t_ang], writes=[t_tq])
        S.op("dve", lambda e: e.scalar_tensor_tensor(out=ang, in0=tq, scalar=-TWO_PI, in1=ang, op0=ALU.mult, op1=ALU.add),
             reads=[t_tq, t_ang], writes=[t_ang])
        S.op("dve", lambda e: e.tensor_scalar(out=tq, in0=ang, scalar1=float(-np.pi), scalar2=None, op0=ALU.is_lt), reads=[t_ang], writes=[t_tq])
        S.op("dve", lambda e: e.scalar_tensor_tensor(out=ang, in0=tq, scalar=TWO_PI, in1=ang, op0=ALU.mult, op1=ALU.add),
             reads=[t_tq, t_ang], writes=[t_ang])
        S.op("act", lambda e: e.activation(out=sinT, in_=ang, func=AF.Sin), reads=[t_ang], writes=[t_cs])
        S.op("dve", lambda e: e.scalar_tensor_tensor(out=tq, in0=ang, scalar=-1.0, in1=ang, op0=ALU.mult, op1=ALU.max), reads=[t_ang], writes=[t_tq])
        hpi = sb("hpi", [128, 1], F32)
        t_hpi = T("hpi")
        S.op("pool", lambda e: e.memset(hpi[:], float(np.pi / 2)), writes=[t_hpi])
        S.op("act", lambda e: e.activation(out=cosT, in_=tq, func=AF.Sin, scale=-1.0, bias=hpi[:, 0:1]), reads=[t_tq, t_hpi], writes=[t_cs])
        barrier()

        pb = [1, 2]
        nstat = [0]

        def norm_mod(src_fn, t_src, w, dst_fn, t_dst, A_fn, B_fn, t_AB, dst_is_f32=False, src3d=None):
            bk = pb[nstat[0] % 2]
            nstat[0] += 1
            if src3d is not None:
                S.op("act", lambda e: e.activation(out=sqb[:, :, 0:w], in_=src3d, func=AF.Square), reads=t_src, writes=[t_sqb])
            else:
                for k in range(KC):
                    S.op("act", lambda e, k=k: e.activation(out=sqb[:, k, 0:w], in_=src_fn(k), func=AF.Square),
                         reads=t_src, writes=[t_sqb])
            mm_group(psum[:, bk, 0:w], [(onesD[:], sqb[:, k, 0:w]) for k in range(KC)], [t_ones, t_sqb], [tP[bk]])
            S.op("act", lambda e: e.activation(out=rstd[:, 0:w], in_=psum[:, bk, 0:w], func=AF.Ln, bias=epsc[:, 0:1]), reads=[tP[bk], t_epsc], writes=[t_rstd])
            S.op("act", lambda e: e.activation(out=rstd[:, 0:w], in_=rstd[:, 0:w], func=AF.Exp, scale=-0.5), reads=[t_rstd], writes=[t_rstd])
            for k in range(KC):
                tt, t_tt = (tmpA, t_tmpA) if k % 2 == 0 else (tmpB, t_tmpB)
                S.op("dve", lambda e, k=k, tt=tt: e.tensor_tensor(out=tt[:, 0:w], in0=src_fn(k), in1=rstd[:, 0:w], op=ALU.mult),
                     reads=t_src + [t_rstd], writes=[t_tt])
                if B_fn is None:
                    S.op("act", lambda e, k=k, tt=tt: e.activation(out=dst_fn(k), in_=tt[:, 0:w], func=AF.Copy, scale=A_fn(k)),
                         reads=[t_tt] + t_AB, writes=t_dst)
                else:
                    S.op("act", lambda e, k=k, tt=tt: e.activation(out=dst_fn(k), in_=tt[:, 0:w], func=AF.Identity,
                                                                   scale=A_fn(k), bias=B_fn(k)),
                         reads=[t_tt] + t_AB, writes=t_dst)

        xall_v = dr["xT_all"].rearrange("(k p) n -> p k n", p=128)
        xprev_v = dr["xT_prev"].rearrange("(k p) n -> p k n", p=128)

        def load_xs(src_v, c0, w, buf=None, t_buf=None):
            buf = xs if buf is None else buf
            t_buf = t_xs if t_buf is None else t_buf
            S.dma("sp", lambda e: e.dma_start(out=buf[:, :, 0:w], in_=src_v[:, :, c0:c0 + w]), writes=[t_buf])

        xs2 = view(R_S + 30720 + 2 * NT * 2 + 8192 + 32 * 130 * 2 + NPB * 1024, 16384, F32).rearrange("p (k n) -> p k n", k=KC)
        t_xs2 = T("xs2")
        xbufs = [(xs, t_xs), (xs2, t_xs2)]

        for ci in range(4):
            c0, w = ci * 512, 512
            xb, t_xb = xbufs[ci % 2]
            load_xs(xprev_v, c0, w, xb, t_xb)
            norm_mod(lambda k, xb=xb: xb[:, k, 0:w], [t_xb], w, lambda k, c0=c0, w=w: hTp[:, k, c0:c0 + w], [t_hp[ci]],
                     lambda k: Amix[:, 0, k:k + 1], lambda k: modv[:, 0, k:k + 1], [t_A, t_mod], src3d=xb[:, :, 0:w])
        for ci, (c0, w) in enumerate(CH):
            xb, t_xb = xbufs[ci % 2]
            load_xs(xall_v, c0, w, xb, t_xb)
            norm_mod(lambda k, xb=xb: xb[:, k, 0:w], [t_xb], w, lambda k, c0=c0, w=w: hT[:, k, c0:c0 + w], [t_h[ci]],
                     lambda k: Amix[:, 0, k:k + 1], lambda k: modv[:, 0, k:k + 1], [t_A, t_mod], src3d=xb[:, :, 0:w])

        wqkv = dr["attn_w_qkv"].rearrange("(k p) n -> p k n", p=128)

        def h_kv(ch, k, a=0, wd=512):
            if ch < 4:
                return hTp[:, k, ch * 512 + a: ch * 512 + a + wd]
            return hT[:, k, (ch - 4) * 512 + a:(ch - 4) * 512 + a + wd]

        def t_hkv(ch):
            return t_hp[ch] if ch < 4 else t_h[ch - 4]

        S.op("pool", lambda e: e.memset(qz[:, :, :], 0.0), writes=t_q)
        pT_i = [0]
        sbank_i = [0]
        gstep = [0]
        rp_i = [0]
        ada_t = [0]
        wa2 = [view(R_S + sl * 8192, 8192, BF16).rearrange("p (k n) -> p k n", k=KC) for sl in range(2)]
        t_wa2 = TL(2, "wa2")

        def ada_task():
            t = ada_t[0]
            if t >= len(ada_rest) + 2:
                return
            ada_t[0] += 1
            if 2 <= t:
                i, pc = ada_rest[t - 2]
                ada_mm(i, pc, wa2[t % 2], t_wa2[t % 2], 3)
            if t < len(ada_rest):
                i, pc = ada_rest[t]
                ada_dma(i, pc, wa2[t % 2], t_wa2[t % 2], extra_w=[t_xs])
        epb_i = [0]
        SB3 = [0, 4, 5]
        deferred = []
        for c in range(KC):
            for wi, col in ((0, c * 128), (2, D + c * 128), (4, 2 * D + c * 128)):
                S.dma("pool", lambda e, wi=wi, col=col: e.dma_start(out=wq5[:, wi], in_=wqkv[:, :, col:col + 128]), writes=[t_w5[wi]])
            for wi in (0, 2):
                for (dst, src, sgn) in ((0, 32, -1.0), (32, 0, 1.0), (64, 96, -1.0), (96, 64, 1.0)):
                    S.op("pool", lambda e, wi=wi, dst=dst, src=src, sgn=sgn: e.tensor_scalar(
                        out=wq5[:, wi + 1, :, dst:dst + 32], in0=wq5[:, wi, :, src:src + 32], scalar1=sgn, scalar2=None, op0=ALU.mult),
                        reads=[t_w5[wi]], writes=[t_w5[wi + 1]])

            def rope_proj(wi, rhs_fn, t_rhs, w, tab0, dst_ap, t_dst, qcol=0):
                ba, bb = (3, 4) if rp_i[0] % 2 == 0 else (0, 1)
                rp_i[0] += 1
                mm_group(psum[:, ba, 0:w], [(wq5[:, wi, k, :], rhs_fn(k)) for k in range(KC)], [t_w5[wi]] + t_rhs, [tP[ba]])
                mm_group(psum[:, bb, 0:w], [(wq5[:, wi + 1, k, :], rhs_fn(k)) for k in range(KC)], [t_w5[wi + 1]] + t_rhs, [tP[bb]])
                S.op("dve", lambda e: e.tensor_tensor(out=tmpA[:, 0:w], in0=psum[:, ba, 0:w], in1=cosT[:, tab0:tab0 + w], op=ALU.mult),
                     reads=[tP[ba], t_cs], writes=[t_tmpA])
                S.op("dve", lambda e: e.tensor_tensor(out=tmpB[:, 0:w], in0=psum[:, bb, 0:w], in1=sinT[:, tab0:tab0 + w], op=ALU.mult),
                     reads=[tP[bb], t_cs], writes=[t_tmpB])
                if dst_ap is None:
                    for jq in range(2):
                        S.op("pool", lambda e, jq=jq: e.tensor_tensor(out=qz[64 * jq:64 * jq + 64, jq, qcol:qcol + w], in0=tmpA[64 * jq:64 * jq + 64, 0:w],
                                                                  in1=tmpB[64 * jq:64 * jq + 64, 0:w], op=ALU.add),
                             reads=[t_tmpA, t_tmpB], writes=t_dst)
                else:
                    S.op("pool", lambda e: e.tensor_tensor(out=dst_ap, in0=tmpA[:, 0:w], in1=tmpB[:, 0:w], op=ALU.add),
                         reads=[t_tmpA, t_tmpB], writes=t_dst)

            for ch in range(8):
                rope_proj(2, lambda k, ch=ch: h_kv(ch, k), [t_hkv(ch)], 512, ch * 512, kT[:, ch * 512:(ch + 1) * 512], [t_k[ch]])
            for ci, (c0, w) in enumerate(CH):
                tab0 = NP + c0 if ci < 4 else NP - HALO
                rope_proj(0, lambda k, c0=c0, w=w: hT[:, k, c0:c0 + w], [t_h[ci]], w, tab0, None, [t_q[ci]], qcol=c0)
            for ch in range(8):
                for tt in range(4):
                    mm_group(psum[:, 5, tt * 128:(tt + 1) * 128],
                             [(h_kv(ch, k, tt * 128, 128), wq5[:, 4, k, :]) for k in range(KC)], [t_w5[4], t_hkv(ch)], [tP[5]])
                S.op("act", lambda e, ch=ch: e.activation(out=Vc[:, ch * 4:(ch + 1) * 4, 0:128],
                                                          in_=psum[:, 5, :].rearrange("p (t n) -> p t n", t=4), func=AF.Copy),
                     reads=[tP[5]], writes=[t_v[ch]])

            steps = []
            for gi, (c0, w) in enumerate(CH):
                if gi < 4:
                    ktiles = [(kt, "prev") for kt in range(16)] + [(16 + kt, "full") for kt in range(4 * gi)] + \
                             [(16 + 4 * gi + t, ("diag", t)) for t in range(4)]
                    nqs = 4
                else:
                    ktiles = [(kt, "prev") for kt in range(15)] + [(15, "hdiag")]
                    nqs = 1
                for j in range(2):
                    for ki, (kt, kind) in enumerate(ktiles):
                        steps.append(dict(gi=gi, c0=c0, w=w, j=j, ki=ki, kt=kt, kind=kind, last=(ki == len(ktiles) - 1),
                                          nqs=nqs, qw=min(w, 128)))

            def emit_qk(sp):
                w, c0, j, kt, kind, gi = sp["w"], sp["c0"], sp["j"], sp["kt"], sp["kind"], sp["gi"]
                pj = slice(64 * j, 64 * j + 64)
                sbk = SB3[sbank_i[0] % 3]
                sbank_i[0] += 1
                pairs = [(kT[:, kt * 128:(kt + 1) * 128], qz[:, j, c0:c0 + w])]
                rds = [t_k[kt // 4], t_q[gi]]
                if isinstance(kind, tuple):
                    pairs.append((small["ident_b"][:], small["masks"][:, kind[1], :]))
                    rds += [tsm["ident_b"], tsm["masks"]]
                elif kind == "hdiag":
                    pairs.append((small["ident_b"][:], small["maskh"][:]))
                    rds += [tsm["ident_b"], tsm["maskh"]]
                mm_group(psum[:, sbk, 0:w], pairs, rds, [tP[sbk]])
                pi = pT_i[0] % NPB
                pT_i[0] += 1
                sp["pi"] = pi
                if kind in ("prev", "hdiag"):
                    S.op("act", lambda e: e.activation(out=pT[pi][:, 0:w], in_=psum[:, sbk, 0:w], func=AF.Exp,
                                                       scale=0.125, bias=small["visb"][:, 0:1]),
                         reads=[tP[sbk], tsm["visb"]], writes=[t_pT[pi]])
                else:
                    S.op("act", lambda e: e.activation(out=pT[pi][:, 0:w], in_=psum[:, sbk, 0:w], func=AF.Exp, scale=0.125),
                         reads=[tP[sbk]], writes=[t_pT[pi]])

            def emit_pv(sp):
                j, kt, ki, last, w, pi = sp["j"], sp["kt"], sp["ki"], sp["last"], sp["w"], sp["pi"]
                obk = 6 + j
                lbk = 1 + j

                def pv(e):
                    e.matmul(psum[:, obk, 0:w], lhsT=Vc[:, kt, 0:128], rhs=pT[pi][:, 0:w], start=(ki == 0), stop=last)
                    return e.matmul(psum[:, lbk, 0:w], lhsT=small["ident_b"][:], rhs=pT[pi][:, 0:w], start=(ki == 0), stop=last)
                S.op("pe", pv, reads=[t_pT[pi], t_v[kt // 4], tsm["ident_b"]], writes=[tP[obk], tP[lbk]])
                if last:
                    while deferred:
                        deferred.pop(0)[1]()
                    S.op("dve", lambda e: e.tensor_copy(out=eO[j][:, 0:w], in_=psum[:, obk, 0:w]),
                         reads=[tP[obk]], writes=[t_eO[j]])
                    S.op("dve", lambda e: e.tensor_scalar(out=eL[j][:, 0:w], in0=psum[:, lbk, 0:w], scalar1=1e-32, scalar2=None, op0=ALU.add),
                         reads=[tP[lbk]], writes=[t_eL[j]])

            def epilogue_tasks(sp, s_end):
                gi, c0, w = sp["gi"], sp["c0"], sp["w"]

                def tA():
                    mm_group(psum[:, 3, 0:w], [(onesF1[:], eL[0][:, 0:w])], [t_ones, t_eL[0]], [tP[3]])
                    S.op("dve", lambda e: e.reciprocal(out=eL[0][:, 0:w], in_=psum[:, 3, 0:w]), reads=[tP[3]], writes=[t_eL[0]])
                    S.op("dve", lambda e: e.tensor_tensor(out=eO[0][:, 0:w], in0=eO[0][:, 0:w], in1=eL[0][:, 0:w], op=ALU.mult),
                         reads=[t_eO[0], t_eL[0]], writes=[t_eO[0]])

                def tB():
                    mm_group(psum[:, 3, 0:w], [(onesF1[:], eL[1][:, 0:w])], [t_ones, t_eL[1]], [tP[3]])
                    S.op("dve", lambda e: e.reciprocal(out=eL[1][:, 0:w], in_=psum[:, 3, 0:w]), reads=[tP[3]], writes=[t_eL[1]])
                    S.op("dve", lambda e: e.scalar_tensor_tensor(out=eO[1][:, 0:w], in0=eO[1][:, 0:w], scalar=neglam[:, 0:1], in1=eL[1][:, 0:w],
                                                                 op0=ALU.mult, op1=ALU.mult), reads=[t_eO[1], t_eL[1], t_lam], writes=[t_eO[1]])
                    S.op("dve", lambda e: e.tensor_tensor(out=eO[0][:, 0:w], in0=eO[0][:, 0:w], in1=eO[1][:, 0:w], op=ALU.add),
                         reads=[t_eO[0], t_eO[1]], writes=[t_eO[0]])
                    S.op("dve", lambda e: e.tensor_tensor(out=asq[:, 0:w], in0=eO[0][:, 0:w], in1=eO[0][:, 0:w], op=ALU.mult), reads=[t_eO[0]], writes=[t_asq])

                def tC():
                    mm_group(psum[:, 3, 0:w], [(ones128[:], asq[:, 0:w])], [t_ones, t_asq], [tP[3]])
                    S.op("act", lambda e: e.activation(out=ers[:, 0:w], in_=psum[:, 3, 0:w], func=AF.Ln, bias=epsc[:, 0:1]),
                         reads=[tP[3], t_epsc], writes=[t_ers])
                    S.op("act", lambda e: e.activation(out=ers[:, 0:w], in_=ers[:, 0:w], func=AF.Exp, scale=-0.5), reads=[t_ers], writes=[t_ers])
                    S.op("dve", lambda e: e.scalar_tensor_tensor(out=oT[:, c, c0:c0 + w], in0=eO[0][:, 0:w], scalar=gsubc[:, 0:1], in1=ers[:, 0:w],
                                                                 op0=ALU.mult, op1=ALU.mult), reads=[t_eO[0], t_ers, t_gsub], writes=[t_oT[gi]])
                return [(s_end + 2, tA), (s_end + 8, tB), (s_end + 14, tC)]

            LOOK = 2
            for idx in range(len(steps) + LOOK):
                while deferred and deferred[0][0] <= idx:
                    deferred.pop(0)[1]()
                gstep[0] += 1
                if gstep[0] % 60 == 30:
                    ada_task()
                if idx < len(steps):
                    emit_qk(steps[idx])
                if idx >= LOOK:
                    sp = steps[idx - LOOK]
                    emit_pv(sp)
                    if sp["last"] and sp["j"] == 1:
                        deferred.extend(epilogue_tasks(sp, idx))
                        deferred.sort(key=lambda t: t[0])
            for _, fl in deferred:
                fl()
            deferred.clear()

        while ada_t[0] < len(ada_rest) + 2:
            ada_task()
        S.op("dve", lambda e: e.scalar_tensor_tensor(out=Affn[:], in0=modv[:, :, 32:40], scalar=1.0, in1=small["gffnT"][:],
                                                     op0=ALU.add, op1=ALU.mult), reads=[t_mod, tsm["gffnT"]], writes=[t_A])
        S.op("dve", lambda e: e.scalar_tensor_tensor(out=Amix[:, 1, :], in0=modv[:, 1, 8:16], scalar=1.0, in1=small["gmixT"][:, 1, :],
                                                     op0=ALU.add, op1=ALU.mult), reads=[t_mod, tsm["gmixT"]], writes=[t_A])
        yb = [4, 5, 6, 7]
        yb_i = [0]

        def proj_residual(w_sb, t_w, rhs_fn, t_rhs, ci, c0, w, Gcol_fn, t_G, src_fn, t_src, bias_fn=None, t_bias=()):
            for d in range(KC):
                bk = yb[yb_i[0] % 4]
                yb_i[0] += 1
                mm_group(psum[:, bk, 0:w], [(w_sb[:, k, d * 128:(d + 1) * 128], rhs_fn(k)) for k in range(KC)], [t_w] + t_rhs, [tP[bk]])
                if bias_fn is None:
                    S.op("dve", lambda e, d=d, bk=bk: e.scalar_tensor_tensor(out=xT[:, d, c0:c0 + w], in0=psum[:, bk, 0:w], scalar=Gcol_fn(d),
                                                                             in1=src_fn(d), op0=ALU.mult, op1=ALU.add),
                         reads=[tP[bk]] + t_G + t_src(d), writes=[t_x[ci][d]])
                else:
                    S.op("act", lambda e, d=d, bk=bk: e.activation(out=tmpA[:, 0:w], in_=psum[:, bk, 0:w], func=AF.Identity, bias=bias_fn(d)),
                         reads=[tP[bk]] + list(t_bias), writes=[t_tmpA])
                    S.op("dve", lambda e, d=d: e.scalar_tensor_tensor(out=xT[:, d, c0:c0 + w], in0=tmpA[:, 0:w], scalar=Gcol_fn(d),
                                                                      in1=src_fn(d), op0=ALU.mult, op1=ALU.add),
                         reads=[t_tmpA] + t_G + t_src(d), writes=[t_x[ci][d]])

        barrier()
        S.dma("pool", lambda e: e.dma_start(out=wo_sb, in_=dr["attn_w_o"].rearrange("(k p) n -> p k n", p=128)), writes=[t_wo])
        for ci, (c0, w) in enumerate(CH):
            load_xs(xall_v, c0, w)
            proj_residual(wo_sb, t_wo, lambda k, c0=c0, w=w: oT[:, k, c0:c0 + w], [t_oT[ci]], ci, c0, w,
                          lambda d: modv[:, 0, 16 + d:17 + d], [t_mod], lambda d, w=w: xs[:, d, 0:w], lambda d: [t_xs])
            if stage > 1:
                norm_mod(lambda k, c0=c0, w=w: xT[:, k, c0:c0 + w], t_x[ci], w, lambda k, c0=c0, w=w: hT[:, k, c0:c0 + w], [t_h[ci]],
                         lambda k: Affn[:, 0, k:k + 1], lambda k: modv[:, 0, 24 + k:25 + k], [t_A, t_mod], src3d=xT[:, :, c0:c0 + w])
        if stage <= 1:
            return finish(nc, S, st, outT, xT, t_x)

        barrier()
        o = R_S + 16384 + 8192 + 3 * 2048
        FGW = 512
        wg_sb = [view(o + s * 8192, 8192, BF16).rearrange("p (k n) -> p k n", k=KC) for s in range(2)]; o += 16384
        wu_sb = [view(o + s * 8192, 8192, BF16).rearrange("p (k n) -> p k n", k=KC) for s in range(2)]; o += 16384
        wd_one = view(o, 8192, BF16).rearrange("p (c n) -> p c n", c=4); o += 8192
        wd_sb = [wd_one, wd_one]
        t_wd1 = T("wd")
        t_wg, t_wu, t_wd = TL(2, "wg"), TL(2, "wu"), [t_wd1, t_wd1]
        sg = [view(o + i * 1024, 1024, BF16) for i in range(2)]; o += 2048
        t_sg = TL(2, "sg")
        actb = [view(o + i * 4096, 4096, BF16).rearrange("p (c n) -> p c n", c=4) for i in range(2)]; o += 8192
        t_act = TL(2, "act")
        FFN_END = o
        assert o <= ARENA_W * 4, o
        ffn_i = [0]
        gu_i = [0]
        act_i = [0]

        def ffn(wg_d, wu_d, wd_d, F, chunks, hg_fn, t_hg, hu_fn, t_hu, G_fn, t_G, tok_gate=None, on_chunk_done=None, first_gu_preloaded=False):
            wgv = wg_d.rearrange("(k p) f -> p k f", p=128)
            wuv = wu_d.rearrange("(k p) f -> p k f", p=128)
            nfg = (F + FGW - 1) // FGW

            def down(sl, nfc, ai, ci, c0, w, is_last_fg):
                for d in range(KC):
                    bk = yb[yb_i[0] % 4]
                    yb_i[0] += 1
                    mm_group(psum[:, bk, 0:w], [(wd_sb[sl][:, fc, d * 128:(d + 1) * 128], actb[ai][:, fc, 0:w]) for fc in range(nfc)],
                             [t_wd[sl], t_act[ai]], [tP[bk]])
                    S.op("dve", lambda e, d=d, bk=bk: e.scalar_tensor_tensor(
                        out=xT[:, d, c0:c0 + w], in0=psum[:, bk, 0:w], scalar=G_fn(d), in1=xT[:, d, c0:c0 + w], op0=ALU.mult, op1=ALU.add),
                        reads=[tP[bk]] + t_G + [t_x[ci][d]], writes=[t_x[ci][d]])
                if is_last_fg and on_chunk_done is not None:
                    on_chunk_done(ci, c0, w)

            pend = None
            for fg in range(nfg):
                f0 = fg * FGW
                fw = min(FGW, F - f0)
                nfc = fw // 128
                sl = ffn_i[0] % 2
                ffn_i[0] += 1
                if not (first_gu_preloaded and fg == 0):
                    S.dma("pool", lambda e, sl=sl, f0=f0, fw=fw: e.dma_start(out=wg_sb[sl][:, :, 0:fw], in_=wgv[:, :, f0:f0 + fw]), writes=[t_wg[sl]])
                    S.dma("pool", lambda e, sl=sl, f0=f0, fw=fw: e.dma_start(out=wu_sb[sl][:, :, 0:fw], in_=wuv[:, :, f0:f0 + fw]), writes=[t_wu[sl]])
                first = True
                for (ci, c0, w) in chunks:
                    ai = act_i[0] % 2
                    act_i[0] += 1
                    for fc in range(nfc):
                        gi_ = gu_i[0] % 2
                        gu_i[0] += 1
                        bg, bu = (0, 2) if gi_ == 0 else (1, 3)
                        mm_group(psum[:, bg, 0:w], [(wg_sb[sl][:, k, fc * 128:(fc + 1) * 128], hg_fn(k, c0, w)) for k in range(KC)],
                                 [t_wg[sl]] + t_hg(ci), [tP[bg]])
                        mm_group(psum[:, bu, 0:w], [(wu_sb[sl][:, k, fc * 128:(fc + 1) * 128], hu_fn(k, c0, w)) for k in range(KC)],
                                 [t_wu[sl]] + t_hu(ci), [tP[bu]])
                        S.op("act", lambda e, gi_=gi_, bg=bg: e.activation(out=sg[gi_][:, 0:w], in_=psum[:, bg, 0:w], func=AF.Silu),
                             reads=[tP[bg]], writes=[t_sg[gi_]])
                        S.op("dve", lambda e, gi_=gi_, bu=bu, ai=ai, fc=fc: e.tensor_tensor(out=actb[ai][:, fc, 0:w], in0=psum[:, bu, 0:w],
                                                                                          in1=sg[gi_][:, 0:w], op=ALU.mult),
                             reads=[tP[bu], t_sg[gi_]], writes=[t_act[ai]])
                        if tok_gate is not None:
                            S.op("dve", lambda e, ai=ai, fc=fc, ci=ci, w=w: e.tensor_tensor(out=actb[ai][:, fc, 0:w], in0=actb[ai][:, fc, 0:w],
                                                                                          in1=tok_gate[0](ci, w), op=ALU.mult),
                                 reads=[t_act[ai]] + tok_gate[1](ci), writes=[t_act[ai]])
                    if pend is not None:
                        down(*pend)
                    if first:
                        S.dma("pool", lambda e, sl=sl, f0=f0, fw=fw, nfc=nfc: e.dma_start(
                            out=wd_sb[sl][:, 0:nfc, :], in_=wd_d[f0:f0 + fw, :].rearrange("(c p) d -> p c d", p=128)), writes=[t_wd[sl]])
                        first = False
                    pend = (sl, nfc, ai, ci, c0, w, fg == nfg - 1)
            if pend is not None:
                down(*pend)

        ffn(dr["ffn_w_gate"], dr["ffn_w_up"], dr["ffn_w_down"], FF0, [(ci, c0, w) for ci, (c0, w) in enumerate(CH)],
            lambda k, c0, w: hT[:, k, c0:c0 + w], lambda ci: [t_h[ci]], lambda k, c0, w: hT[:, k, c0:c0 + w], lambda ci: [t_h[ci]],
            lambda d: modv[:, 0, 40 + d:41 + d], [t_mod],
            on_chunk_done=(None if stage <= 2 else (lambda ci, c0, w: norm_mod(
                lambda k: xT[:, k, c0:c0 + w], t_x[ci], w, lambda k: hT[:, k, c0:c0 + w], [t_h[ci]],
                lambda k: Amix[:, 1, k:k + 1], lambda k: modv[:, 1, k:k + 1], [t_A, t_mod], src3d=xT[:, :, c0:c0 + w]))))
        if stage <= 2:
            return finish(nc, S, st, outT, xT, t_x)

        barrier()
        o = R_S + 16384 + 8192 + 3 * 2048
        UW = HALO + NO
        uT = view(o, KC * UW * 2, BF16).rearrange("p (k n) -> p k n", k=KC); o += KC * UW * 2
        t_u = TL(KC, "u")
        wpw2 = [view(o + i * 4096, 4096, BF16).rearrange("p (k n) -> p k n", k=KC) for i in range(2)]; o += 8192
        t_wpw2 = TL(2, "wpw")
        dg = [view(o + i * 7936, 7936, BF16).rearrange("p (j n) -> p j n", j=31) for i in range(2)]; o += 2 * 7936
        t_dg = TL(2, "dg")
        sgl = view(o, 2048, F32); o += 2048
        t_sgl = T("sgl")
        lnm = view(o, 2048, F32); o += 2048
        lnr = view(o, 2048, F32); o += 2048
        t_lnm, t_lnr = T("lnm"), T("lnr")
        assert o <= ARENA_W * 4, o
        vT = xs
        t_vT = TL(KC, "v")
        vsq = sqb
        t_vsq = t_sqb
        zT = sqb
        t_z = t_sqb
        pw1v = dr["conv_w_pw1"].rearrange("(k p) n -> p k n", p=128)

        for fcn in range(KC):
            wpw, t_wpw = wpw2[fcn % 2], t_wpw2[fcn % 2]
            S.dma("pool", lambda e, fcn=fcn: e.dma_start(out=wpw[:, :, 0:128], in_=pw1v[:, :, fcn * 128:(fcn + 1) * 128]), writes=[t_wpw])
            S.dma("pool", lambda e, fcn=fcn: e.dma_start(out=wpw[:, :, 128:256], in_=pw1v[:, :, D + fcn * 128:D + (fcn + 1) * 128]), writes=[t_wpw])
            for ci, (c0, w) in enumerate(CH):
                pa, pg = (0, 1) if (ci % 2 == 0) else (2, 3)
                mm_group(psum[:, pa, 0:w], [(wpw[:, k, 0:128], hT[:, k, c0:c0 + w]) for k in range(KC)], [t_wpw, t_h[ci]], [tP[pa]])
                mm_group(psum[:, pg, 0:w], [(wpw[:, k, 128:256], hT[:, k, c0:c0 + w]) for k in range(KC)], [t_wpw, t_h[ci]], [tP[pg]])
                S.op("act", lambda e, fcn=fcn, w=w: e.activation(out=sgl[:, 0:w], in_=psum[:, pg, 0:w], func=AF.Sigmoid,
                                                               bias=small["bpw1T"][:, 8 + fcn:9 + fcn]), reads=[tP[pg], tsm["bpw1T"]], writes=[t_sgl])
                if ci < 4:
                    u0 = HALO + c0
                    S.op("dve", lambda e, fcn=fcn, w=w, u0=u0: e.scalar_tensor_tensor(
                        out=uT[:, fcn, u0:u0 + w], in0=psum[:, pa, 0:w], scalar=small["bpw1T"][:, fcn:fcn + 1], in1=sgl[:, 0:w],
                        op0=ALU.add, op1=ALU.mult), reads=[tP[pa], tsm["bpw1T"], t_sgl], writes=[t_u[fcn]])
                else:
                    S.op("dve", lambda e, fcn=fcn, w=w: e.scalar_tensor_tensor(
                        out=tmpA[:, 0:w], in0=psum[:, pa, 0:w], scalar=small["bpw1T"][:, fcn:fcn + 1], in1=sgl[:, 0:w],
                        op0=ALU.add, op1=ALU.mult), reads=[tP[pa], tsm["bpw1T"], t_sgl], writes=[t_tmpA])
                    S.op("dve", lambda e, fcn=fcn, w=w: e.tensor_scalar(out=uT[:, fcn, 0:w], in0=tmpA[:, 0:w], scalar1=small["hflag"][:, 0:1],
                                                                      scalar2=None, op0=ALU.mult), reads=[t_tmpA, tsm["hflag"]], writes=[t_u[fcn]])
        w2_sb = view(R_H, 16384, BF16).rearrange("p (k n) -> p k n", k=KC)
        t_w2 = T("w2")
        barrier()
        S.dma("pool", lambda e: e.dma_start(out=w2_sb, in_=dr["conv_w_pw2"].rearrange("(k p) n -> p k n", p=128)), writes=[t_w2])
        dg_i = [0]
        for ci, (c0, w) in enumerate(CHO):
            for fcn in range(KC):
                di = dg_i[0] % 2
                dg_i[0] += 1
                S.op("dve", lambda e, fcn=fcn, di=di: e.tensor_tensor(
                    out=dg[di][:, :, :], in0=small["ident_b"][:].rearrange("p (o n) -> p o n", o=1).to_broadcast([128, 31, 128]),
                    in1=small["wdwT"][:, fcn, :].rearrange("p (j o) -> p j o", o=1).to_broadcast([128, 31, 128]), op=ALU.mult),
                    reads=[tsm["ident_b"], tsm["wdwT"]], writes=[t_dg[di]])
                bk = fcn % 2
                mm_group(psum[:, bk, 0:w], [(dg[di][:, j, :], uT[:, fcn, c0 + 2 + j:c0 + 2 + j + w]) for j in range(31)],
                         [t_dg[di], t_u[fcn]], [tP[bk]])
                S.op("act", lambda e, fcn=fcn, bk=bk, w=w: e.activation(out=vT[:, fcn, 0:w], in_=psum[:, bk, 0:w], func=AF.Identity,
                                                                      bias=small["bdwT"][:, fcn:fcn + 1]), reads=[tP[bk], tsm["bdwT"]], writes=[t_vT[fcn]])
                S.op("act", lambda e, fcn=fcn, w=w: e.activation(out=vsq[:, fcn, 0:w], in_=vT[:, fcn, 0:w], func=AF.Square),
                     reads=[t_vT[fcn]], writes=[t_vsq])
            mm_group(psum[:, 2, 0:w], [(onesF[:], vT[:, k, 0:w]) for k in range(KC)], [t_ones] + t_vT, [tP[2]])
            mm_group(psum[:, 3, 0:w], [(onesD[:], vsq[:, k, 0:w]) for k in range(KC)], [t_ones, t_vsq], [tP[3]])
            S.op("act", lambda e, w=w: e.activation(out=lnm[:, 0:w], in_=psum[:, 2, 0:w], func=AF.Copy), reads=[tP[2]], writes=[t_lnm])
            S.op("dve", lambda e, w=w: e.tensor_tensor(out=lnr[:, 0:w], in0=lnm[:, 0:w], in1=lnm[:, 0:w], op=ALU.mult), reads=[t_lnm], writes=[t_lnr])
            S.op("dve", lambda e, w=w: e.tensor_tensor(out=lnr[:, 0:w], in0=psum[:, 3, 0:w], in1=lnr[:, 0:w], op=ALU.subtract), reads=[tP[3], t_lnr], writes=[t_lnr])
            S.op("act", lambda e, w=w: e.activation(out=lnr[:, 0:w], in_=lnr[:, 0:w], func=AF.Ln, bias=epsc[:, 0:1]), reads=[t_lnr, t_epsc], writes=[t_lnr])
            S.op("act", lambda e, w=w: e.activation(out=lnr[:, 0:w], in_=lnr[:, 0:w], func=AF.Exp, scale=-0.5), reads=[t_lnr], writes=[t_lnr])
            for fcn in range(KC):
                S.op("dve", lambda e, fcn=fcn, w=w: e.tensor_tensor(out=vT[:, fcn, 0:w], in0=vT[:, fcn, 0:w], in1=lnm[:, 0:w], op=ALU.subtract),
                     reads=[t_vT[fcn], t_lnm], writes=[t_vT[fcn]])
                S.op("dve", lambda e, fcn=fcn, w=w: e.tensor_tensor(out=vT[:, fcn, 0:w], in0=vT[:, fcn, 0:w], in1=lnr[:, 0:w], op=ALU.mult),
                     reads=[t_vT[fcn], t_lnr], writes=[t_vT[fcn]])
                S.op("act", lambda e, fcn=fcn, w=w: e.activation(out=zT[:, fcn, 0:w], in_=vT[:, fcn, 0:w], func=AF.Silu,
                                                               scale=small["lngT"][:, fcn:fcn + 1], bias=small["lnbT"][:, fcn:fcn + 1]),
                     reads=[t_vT[fcn], tsm["lngT"], tsm["lnbT"]], writes=[t_z])
            proj_residual(w2_sb, t_w2, lambda k, w=w: zT[:, k, 0:w], [t_z], ci, c0, w,
                          lambda d: modv[:, 1, 16 + d:17 + d], [t_mod], lambda d, c0=c0, w=w: xT[:, d, c0:c0 + w], lambda d, ci=ci: [t_x[ci][d]],
                          bias_fn=lambda d: small["bpw2T"][:, d:d + 1], t_bias=[tsm["bpw2T"]])
        if stage <= 3:
            return finish(nc, S, st, outT, xT, t_x)

        barrier()
        o = FFN_END
        hf = xs
        t_hf = t_xs
        gT = view(o, NO * 4, F32); o += NO * 4
        t_gT = T("gT")
        Gb = view(o, 4096, BF16).rearrange("p (c n) -> p c n", c=4); o += 4096
        t_Gb = TL(4, "Gb")
        wr = view(o, 256, F32).rearrange("p (k n) -> p k n", k=KC); o += 256
        t_wr = T("wr")
        assert o <= ARENA_W * 4, o
        S.dma("sp", lambda e: e.dma_start(out=wr, in_=dr["moe_w_router"].rearrange("(k p) n -> p k n", p=128)), writes=[t_wr])
        sl0 = ffn_i[0] % 2
        S.dma("pool", lambda e: e.dma_start(out=wg_sb[sl0][:, :, 0:FGW], in_=dr["moe_w_gate"][0].rearrange("(k p) f -> p k f", p=128)[:, :, 0:FGW]), writes=[t_wg[sl0]])
        S.dma("pool", lambda e: e.dma_start(out=wu_sb[sl0][:, :, 0:FGW], in_=dr["moe_w_up"][0].rearrange("(k p) f -> p k f", p=128)[:, :, 0:FGW]), writes=[t_wu[sl0]])
        for ci, (c0, w) in enumerate(CHO):
            norm_mod(lambda k, c0=c0, w=w: xT[:, k, c0:c0 + w], t_x[ci], w, lambda k, w=w: hf[:, k, 0:w], [t_hf],
                     lambda k: Affn[:, 1, k:k + 1], lambda k: modv[:, 1, 24 + k:25 + k], [t_A, t_mod], src3d=xT[:, :, c0:c0 + w])
            S.op("dve", lambda e, c0=c0, w=w: e.tensor_copy(out=hT[:, :, c0:c0 + w], in_=hf[:, :, 0:w]), reads=[t_hf], writes=[t_h[ci]])
            for tt in range(4):
                t16 = ci * 4 + tt
                mm_group(psum[:, 0, t16 * 8:(t16 + 1) * 8], [(hf[:, k, tt * 128:(tt + 1) * 128], wr[:, k, :]) for k in range(KC)],
                         [t_hf, t_wr], [tP[0]])
        rsc = view(R_S + 73728, 4096, F32)
        t_rsc = T("rsc")
        Lf, EQf, L2f, Ef, Gf = (rsc[:, i * 128:(i + 1) * 128] for i in range(5))
        smx = rsc[:, 640:704]
        r3 = lambda ap: ap.rearrange("p (t e) -> p t e", e=8)
        bc = lambda ap: ap.rearrange("p (t o) -> p t o", o=1).to_broadcast([128, 16, 8])
        l1, l2, den, rden = smx[:, 0:16], smx[:, 16:32], smx[:, 32:48], smx[:, 48:64]
        RS = dict(reads=[t_rsc], writes=[t_rsc])
        S.op("dve", lambda e: e.tensor_copy(out=Lf, in_=psum[:, 0, 0:128]), reads=[tP[0]], writes=[t_rsc])
        S.op("dve", lambda e: e.tensor_reduce(out=l1, in_=r3(Lf), axis=AX.X, op=ALU.max), **RS)
        S.op("dve", lambda e: e.tensor_tensor(out=r3(EQf), in0=r3(Lf), in1=bc(l1), op=ALU.is_equal), **RS)
        S.op("dve", lambda e: e.scalar_tensor_tensor(out=L2f, in0=EQf, scalar=-1e30, in1=Lf, op0=ALU.mult, op1=ALU.add), **RS)
        S.op("dve", lambda e: e.tensor_reduce(out=l2, in_=r3(L2f), axis=AX.X, op=ALU.max), **RS)
        S.op("dve", lambda e: e.tensor_tensor(out=r3(EQf), in0=r3(Lf), in1=bc(l2), op=ALU.is_ge), **RS)
        S.op("dve", lambda e: e.tensor_tensor(out=r3(L2f), in0=r3(Lf), in1=bc(l1), op=ALU.subtract), **RS)
        S.op("act", lambda e: e.activation(out=Ef, in_=L2f, func=AF.Exp), **RS)
        S.op("dve", lambda e: e.tensor_tensor(out=Ef, in0=Ef, in1=EQf, op=ALU.mult), **RS)
        S.op("dve", lambda e: e.tensor_reduce(out=den, in_=r3(Ef), axis=AX.X, op=ALU.add), **RS)
        S.op("dve", lambda e: e.reciprocal(out=rden, in_=den), **RS)
        S.op("dve", lambda e: e.tensor_tensor(out=r3(Gf), in0=r3(Ef), in1=bc(rden), op=ALU.mult), **RS)
        for ci in range(4):
            def trs(e, ci=ci):
                ins = None
                for tt in range(4):
                    t16 = ci * 4 + tt
                    ins = e.transpose(out=psum[0:8, 4 + ci, tt * 128:(tt + 1) * 128], in_=Gf[:, t16 * 8:(t16 + 1) * 8], identity=small["ident_f"][:])
                return ins
            S.op("pe", trs, reads=[t_rsc, tsm["ident_f"]], writes=[tP[4 + ci]])
            S.op("dve", lambda e, ci=ci: e.tensor_copy(out=gT[0:8, ci * 512:(ci + 1) * 512], in_=psum[0:8, 4 + ci, 0:512]),
                 reads=[tP[4 + ci]], writes=[t_gT])
        barrier()
        moe_g, moe_u, moe_d = dr["moe_w_gate"], dr["moe_w_up"], dr["moe_w_down"]
        ov = outT.rearrange("(k p) n -> p k n", p=128)
        t_out = T("out")
        out_sv = []

        def final_chunk(ci, c0, w):
            bk = 1 + ci % 2
            S.op("act", lambda e: e.activation(out=sqb[:, :, 0:w], in_=xT[:, :, c0:c0 + w], func=AF.Square), reads=t_x[ci], writes=[t_sqb])
            mm_group(psum[:, bk, 0:w], [(onesD[:], sqb[:, k, 0:w]) for k in range(KC)], [t_ones, t_sqb], [tP[bk]])
            S.op("act", lambda e: e.activation(out=rstd[:, 0:w], in_=psum[:, bk, 0:w], func=AF.Ln, bias=epsc[:, 0:1]),
                 reads=[tP[bk], t_epsc], writes=[t_rstd])
            S.op("act", lambda e: e.activation(out=rstd[:, 0:w], in_=rstd[:, 0:w], func=AF.Exp, scale=-0.5), reads=[t_rstd], writes=[t_rstd])
            for k in range(KC):
                S.op("dve", lambda e, k=k: e.scalar_tensor_tensor(out=xs[:, k, 0:w], in0=xT[:, k, c0:c0 + w], scalar=small["gfinT"][:, k:k + 1],
                                                                 in1=rstd[:, 0:w], op0=ALU.mult, op1=ALU.mult),
                     reads=t_x[ci] + [tsm["gfinT"], t_rstd], writes=[t_xs])
            out_sv.append(S.dma("sp", lambda e: e.dma_start(out=ov[:, :, c0:c0 + w], in_=xs[:, :, 0:w]), reads=[t_xs], sem_t=t_out))
        for ex_i in range(NE):
            for ci, (c0, w) in enumerate(CHO):
                bk = 2 + (ci % 2)
                S.op("pe", lambda e, ex_i=ex_i, bk=bk, c0=c0, w=w: e.matmul(psum[:, bk, 0:w], lhsT=small["sel"][:, ex_i, :], rhs=gT[0:8, c0:c0 + w],
                                                                        start=True, stop=True), reads=[tsm["sel"], t_gT], writes=[tP[bk]])
                S.op("act", lambda e, ci=ci, bk=bk, w=w: e.activation(out=Gb[:, ci, 0:w], in_=psum[:, bk, 0:w], func=AF.Copy),
                     reads=[tP[bk]], writes=[t_Gb[ci]])
            ffn(moe_g[ex_i], moe_u[ex_i], moe_d[ex_i], FFE, [(ci, c0, w) for ci, (c0, w) in enumerate(CHO)],
                lambda k, c0, w: hT[:, k, c0:c0 + w], lambda ci: [t_h[ci]], lambda k, c0, w: hT[:, k, c0:c0 + w], lambda ci: [t_h[ci]],
                lambda d: modv[:, 1, 40 + d:41 + d], [t_mod], tok_gate=(lambda ci, w: Gb[:, ci, 0:w], lambda ci: [t_Gb[ci]]),
                on_chunk_done=(final_chunk if ex_i == NE - 1 else None), first_gu_preloaded=(ex_i == 0))
        assert len(out_sv) == 4
        S.final_waits.append(out_sv[-1])
        S.emit()
        return nc


def finish(nc, S, st, outT, xT, t_x, final=None):
    ov = outT.rearrange("(k p) n -> p k n", p=128)
    t_out = T("out")
    sv = None
    if final is None:
        for ci, (c0, w) in enumerate(CHO):
            sv = S.dma("sp", lambda e, c0=c0, w=w: e.dma_start(out=ov[:, :, c0:c0 + w], in_=xT[:, :, c0:c0 + w]), reads=t_x[ci], sem_t=t_out)
    else:
        (xs, t_xs, sqb, t_sqb, rstd, t_rstd, tmpA, t_tmpA, tmpB, t_tmpB, onesD, t_ones, small, tsm, psum, tP, epsc, t_epsc) = final
        for ci, (c0, w) in enumerate(CHO):
            bk = 1 + ci % 2
            for k in range(KC):
                S.op("act", lambda e, k=k, c0=c0, w=w: e.activation(out=sqb[:, k, 0:w], in_=xT[:, k, c0:c0 + w], func=AF.Square),
                     reads=t_x[ci], writes=[t_sqb])

            def fn(e, bk=bk, w=w):
                ins = None
                for k in range(KC):
                    ins = e.matmul(psum[:, bk, 0:w], lhsT=onesD[:], rhs=sqb[:, k, 0:w], start=(k == 0), stop=(k == KC - 1))
                return ins
            S.op("pe", fn, reads=[t_ones, t_sqb], writes=[tP[bk]])
            S.op("act", lambda e, bk=bk, w=w: e.activation(out=rstd[:, 0:w], in_=psum[:, bk, 0:w], func=AF.Sqrt, bias=epsc[:, 0:1]), reads=[tP[bk], t_epsc], writes=[t_rstd])
            S.op("dve", lambda e, w=w: e.reciprocal(out=rstd[:, 0:w], in_=rstd[:, 0:w]), reads=[t_rstd], writes=[t_rstd])
            for k in range(KC):
                S.op("dve", lambda e, k=k, c0=c0, w=w: e.scalar_tensor_tensor(out=xs[:, k, 0:w], in0=xT[:, k, c0:c0 + w], scalar=small["gfinT"][:, k:k + 1],
                                                                             in1=rstd[:, 0:w], op0=ALU.mult, op1=ALU.mult),
                     reads=t_x[ci] + [tsm["gfinT"], t_rstd], writes=[t_xs])
            sv = S.dma("sp", lambda e, c0=c0, w=w: e.dma_start(out=ov[:, :, c0:c0 + w], in_=xs[:, :, 0:w]), reads=[t_xs], sem_t=t_out)
    S.final_waits.append(sv)
    S.emit()
    return nc


def _consts():
    bf = ml_dtypes.bfloat16
    c = {}
    c["ident_b"] = np.eye(128, dtype=np.float32).astype(bf)
    c["ident_f"] = np.eye(128, dtype=np.float32)
    kk = np.arange(128)[:, None]
    qq = np.arange(512)[None, :]
    c["masks"] = np.stack([np.where(t * 128 + kk > qq, NEG, 0.0) for t in range(4)], axis=1).astype(np.float32).astype(bf)
    c["maskh"] = np.where(kk > 96 + np.arange(HALO)[None, :], NEG, 0.0).astype(np.float32).astype(bf)
    half = 32
    inv = (np.float32(10000.0) ** (-np.arange(half, dtype=np.float32) / np.float32(half))).astype(np.float32)
    c["invf"] = np.tile(inv, 4).reshape(128, 1).astype(np.float32)
    sel = np.zeros((8, NE, 128), np.float32)
    for e in range(NE):
        sel[e, e, :] = 1.0
    c["sel"] = sel
    return c


def _colT(v, n):
    return np.ascontiguousarray(np.asarray(v, np.float32).reshape(n, 128).T)


def make_in_maps(inp):
    cst = _consts()
    x = np.asarray(inp["x"], np.float32)
    pos = np.asarray(inp["positions"], np.int32)
    shared = {n: np.ascontiguousarray(np.asarray(inp[n], np.float32).reshape(sh)) for n, sh in W_SPECS}
    sm = dict(cst)
    sm["badaT"] = np.ascontiguousarray(np.asarray(inp["b_ada"], np.float32).reshape(2, 48, 128).transpose(2, 0, 1))
    sm["gmixT"] = np.ascontiguousarray(np.asarray(inp["norm_mix_g"], np.float32).reshape(2, KC, 128).transpose(2, 0, 1))
    sm["gffnT"] = np.ascontiguousarray(np.asarray(inp["norm_ffn_g"], np.float32).reshape(2, KC, 128).transpose(2, 0, 1))
    sm["gfinT"] = _colT(inp["final_g"], KC)
    lam = np.stack([np.asarray(inp[k], np.float32)[0] for k in ("lam_q1", "lam_k1", "lam_q2", "lam_k2")], 0)
    sm["lamv"] = np.ascontiguousarray(np.broadcast_to(lam[None], (128, 4, 64)))
    sm["gsub"] = np.ascontiguousarray(np.asarray(inp["attn_subln_g"], np.float32)[0].reshape(128, 1))
    sm["bpw1T"] = _colT(np.asarray(inp["conv_b_pw1"])[0], 16)
    sm["wdwT"] = np.ascontiguousarray(np.asarray(inp["conv_w_dw"], np.float32)[0].reshape(31, KC, 128).transpose(2, 1, 0))
    sm["bdwT"] = _colT(np.asarray(inp["conv_b_dw"])[0], KC)
    sm["lngT"] = _colT(np.asarray(inp["conv_ln_g"])[0], KC)
    sm["lnbT"] = _colT(np.asarray(inp["conv_ln_b"])[0], KC)
    sm["bpw2T"] = _colT(np.asarray(inp["conv_b_pw2"])[0], KC)
    maps = []
    for core in range(8):
        b, h = core // 2, core % 2
        m = dict(shared)
        m.update(sm)
        own = x[b, h * NO:(h + 1) * NO, :]
        if h == 1:
            prev = x[b, 0:NP, :]
            halo = prev[NP - HALO:, :]
            ppos = pos[b, 0:NP]
        else:
            prev = np.zeros((NP, D), np.float32)
            halo = np.zeros((HALO, D), np.float32)
            ppos = np.zeros((NP,), np.int32)
        m["xT_all"] = np.ascontiguousarray(np.concatenate([own, halo], 0).T)
        m["xT_prev"] = np.ascontiguousarray(prev.T)
        m["pos_kv"] = np.ascontiguousarray(np.concatenate([ppos, pos[b, h * NO:(h + 1) * NO]])[None, :].astype(np.int32))
        m["cT"] = _colT(np.asarray(inp["c"], np.float32)[b], KC)
        m["visb"] = np.full((128, 1), 0.0 if h == 1 else NEG, np.float32)
        m["hflag"] = np.full((128, 1), 1.0 if h == 1 else 0.0, np.float32)
        maps.append(m)
    return maps


_NC_CACHE = {}


def run(inp, stage=99):
    if stage not in _NC_CACHE:
        _NC_CACHE[stage] = build_program(stage)
    nc = _NC_CACHE[stage]
    maps = make_in_maps(inp)
    res = run_bass_kernel_spmd(nc, maps, core_ids=list(range(8)))
    out = np.empty((4, 2 * NO, D), np.float32)
    for core in range(8):
        b, h = core // 2, core % 2
        out[b, h * NO:(h + 1) * NO, :] = res.results[core]["outT"].T
    return out


def kernel(**inputs):
    return run(inputs, 99)
```

```python
from contextlib import ExitStack
import math
import numpy as np
import ml_dtypes
import concourse.bass as bass
import concourse.mybir as mybir
from concourse.bass_utils import run_bass_kernel_spmd

F32 = mybir.dt.float32
BF16 = mybir.dt.bfloat16
I32 = mybir.dt.int32
AF = mybir.ActivationFunctionType
ALU = mybir.AluOpType
AX = mybir.AxisListType

SEM_LIM = 4000
D = 1024
NO = 2048
HALO = 32
NT = NO + HALO
NP = 2048
KC = 8
FF0 = 2816
FFE = 3584
NE = 8
EPS = 1e-6
NEG = -30000.0
CH = [(0, 512), (512, 512), (1024, 512), (1536, 512), (2048, 32)]
CHO = CH[:4]
LAMBDA_INIT0 = 0.8 - 0.6 * math.exp(-0.3 * 0)


import types


def freeze(fn, depth=0):
    if fn is None or not isinstance(fn, types.FunctionType) or fn.__closure__ is None or depth > 4:
        return fn
    cells = []
    for c in fn.__closure__:
        try:
            v = c.cell_contents
        except ValueError:
            cells.append(c)
            continue
        if isinstance(v, types.FunctionType):
            v = freeze(v, depth + 1)
        elif isinstance(v, tuple) and any(isinstance(x, types.FunctionType) for x in v):
            v = tuple(freeze(x, depth + 1) for x in v)
        cells.append(types.CellType(v))
    g = types.FunctionType(fn.__code__, fn.__globals__, fn.__name__, fn.__defaults__, tuple(cells))
    g.__kwdefaults__ = fn.__kwdefaults__
    return g


class T:
    __slots__ = ("name", "w", "r", "dsem", "dcount")

    def __init__(self, name=""):
        self.name = name
        self.w = None
        self.r = {}
        self.dsem = None
        self.dcount = 0


def TL(n, name=""):
    return [T(f"{name}{i}") for i in range(n)]


class Sched:
    ENGS = ("pe", "act", "dve", "pool", "sp")

    def __init__(self, nc, stack):
        self.nc = nc
        self.stack = stack
        self.ops = {e: [] for e in self.ENGS}
        self.count = {e: 0 for e in self.ENGS}
        self.esems = {e: [] for e in self.ENGS}
        self.seen = {e: {} for e in self.ENGS}
        self.nsem = 0
        self.final_waits = []
        self.dsems = []

    def new_sem(self, name):
        self.nsem += 1
        return self.stack.enter_context(self.nc.semaphore(f"{name}_{self.nsem}"))

    def _eng_sem(self, e, n):
        k = (n - 1) // SEM_LIM
        while len(self.esems[e]) <= k:
            self.esems[e].append(self.new_sem(f"p_{e}"))
        return self.esems[e][k], ((n - 1) % SEM_LIM) + 1

    def _deps(self, e, reads, writes):
        deps = {}

        def add(s, v):
            if deps.get(s, 0) < v:
                deps[s] = v
        for t in reads:
            if t.w is not None:
                add(*t.w)
        for t in writes:
            if t.w is not None:
                add(*t.w)
            for s, v in t.r.items():
                add(s, v)
        out = []
        own = self.esems[e]
        for s, v in deps.items():
            if e == "pe" and any(s is o for o in own):
                continue
            if self.seen[e].get(s, 0) >= v:
                continue
            self.seen[e][s] = v
            out.append((s, v))
        return out

    def _mark(self, sv, reads, writes):
        for t in reads:
            if t.r.get(sv[0], 0) < sv[1]:
                t.r[sv[0]] = sv[1]
        for t in writes:
            t.w = sv
            t.r = {}

    def op(self, e, fn, reads=(), writes=()):
        waits = self._deps(e, reads, writes)
        self.count[e] += 1
        sv = self._eng_sem(e, self.count[e])
        self.ops[e].append((waits, freeze(fn), (sv[0], 1)))
        self._mark(sv, reads, writes)
        return sv

    def dma(self, q, fn, reads=(), writes=(), sem_t=None):
        waits = self._deps(q, reads, writes)
        if sem_t is None:
            sem_t = writes[0] if writes else reads[0]
        if sem_t.dsem is None:
            sem_t.dsem = self.new_sem("d")
            self.dsems.append(sem_t)
        sem_t.dcount += 16
        sv = (sem_t.dsem, sem_t.dcount)
        self.ops[q].append((waits, freeze(fn), (sv[0], 16)))
        self._mark(sv, reads, writes)
        return sv

    def emit(self):
        nc = self.nc
        ops = self.ops
        finals = self.final_waits

        def run(eng, lst, extra=()):
            for waits, fn, inc in lst:
                for s, v in waits:
                    eng.wait_ge(s, v)
                if fn is None:
                    continue
                ins = fn(eng)
                ins.then_inc(inc[0], inc[1])
            for s, v in extra:
                eng.wait_ge(s, v)

        with nc.Block() as block:
            @block.tensor
            def _(eng):
                run(eng, ops["pe"])

            @block.scalar
            def _(eng):
                run(eng, ops["act"])

            @block.vector
            def _(eng):
                run(eng, ops["dve"])

            @block.gpsimd
            def _(eng):
                run(eng, ops["pool"])

            @block.sync
            def _(eng):
                run(eng, ops["sp"], finals)


W_SPECS = [
    ("w_ada", [2, D, 6 * D]), ("attn_w_qkv", [D, 3 * D]), ("attn_w_o", [D, D]),
    ("ffn_w_gate", [D, FF0]), ("ffn_w_up", [D, FF0]), ("ffn_w_down", [FF0, D]),
    ("conv_w_pw1", [D, 2 * D]), ("conv_w_pw2", [D, D]),
    ("moe_w_router", [D, NE]), ("moe_w_gate", [NE, D, FFE]), ("moe_w_up", [NE, D, FFE]),
    ("moe_w_down", [NE, FFE, D]),
]
S_SPECS = [
    ("xT_all", [D, NT], F32), ("xT_prev", [D, NP], F32),
    ("pos_kv", [1, NP + NO], I32),
    ("cT", [128, KC], F32), ("badaT", [128, 2, 48], F32),
    ("gmixT", [128, 2, KC], F32), ("gffnT", [128, 2, KC], F32), ("gfinT", [128, KC], F32),
    ("lamv", [128, 4, 64], F32), ("gsub", [128, 1], F32),
    ("bpw1T", [128, 16], F32), ("wdwT", [128, KC, 31], F32), ("bdwT", [128, KC], F32),
    ("lngT", [128, KC], F32), ("lnbT", [128, KC], F32), ("bpw2T", [128, KC], F32),
    ("invf", [128, 1], F32), ("visb", [128, 1], F32), ("hflag", [128, 1], F32),
    ("ident_b", [128, 128], BF16), ("ident_f", [128, 128], F32),
    ("masks", [128, 4, 512], BF16), ("maskh", [128, HALO], BF16), ("sel", [8, NE, 128], F32),
]


def build_program(stage=99):
    nc = bass.Bass("TRN2", target_bir_lowering=False)
    dr = {}
    for n, sh in W_SPECS:
        dr[n] = nc.dram_tensor(n, sh, F32, kind="ExternalInput").ap()
    for n, sh, dt in S_SPECS:
        dr[n] = nc.dram_tensor(n, sh, dt, kind="ExternalInput").ap()
    outT = nc.dram_tensor("outT", [D, NO], F32, kind="ExternalOutput").ap()

    with ExitStack() as st:
        S = Sched(nc, st)

        def sb(name, shape, dt):
            return st.enter_context(nc.sbuf_tensor(name, shape, dt))

        small = {}
        tsm = {}
        for n, sh, dt in S_SPECS:
            if n in ("xT_all", "xT_prev", "pos_kv"):
                continue
            small[n] = sb("s_" + n, sh, dt)
            tsm[n] = T(n)
            S.dma("sp", lambda e, n=n: e.dma_start(out=small[n][:], in_=dr[n]), writes=[tsm[n]])
        onesD = sb("onesD", [128, 128], BF16)
        t_ones = T("ones")
        S.op("pool", lambda e: e.memset(onesD[:], 1.0 / D), writes=[t_ones])
        epsc = sb("epsc", [128, 1], F32)
        t_epsc = T("epsc")
        S.op("pool", lambda e: e.memset(epsc[:], EPS), writes=[t_epsc])
        onesF = sb("onesF", [128, 128], F32)
        S.op("pool", lambda e: e.memset(onesF[:], 1.0 / D), writes=[t_ones])
        modv = sb("modv", [128, 2, 48], F32)
        t_mod = T("mod")
        Amix = sb("Amix", [128, 2, KC], F32)
        Affn = sb("Affn", [128, 2, KC], F32)
        t_A = T("A")
        cTb = sb("cTb", [128, KC], BF16)
        t_cTb = T("cTb")
        neglam = sb("neglam", [128, 1], F32)
        t_lam = T("lam")
        gsubc = sb("gsubc", [128, 1], F32)
        t_gsub = T("gsubc")
        onesF1 = sb("onesF1", [128, 128], F32)
        ones128 = sb("ones128", [128, 128], BF16)
        S.op("pool", lambda e: e.memset(onesF1[:], 1.0), writes=[t_ones])
        S.op("pool", lambda e: e.memset(ones128[:], 1.0 / 128), writes=[t_ones])

        ARENA_W = 48704
        arena = sb("arena", [128, ARENA_W], F32)
        psum = st.enter_context(nc.psum_tensor("psum", [128, 8, 512], F32))
        tP = TL(8, "ps")

        def view(off, nbytes, dt):
            assert off % 4 == 0 and nbytes % 4 == 0 and off + nbytes <= ARENA_W * 4, (off, nbytes)
            v = arena[:, off // 4:(off + nbytes) // 4]
            return v if dt == F32 else v.bitcast(dt)

        def barrier():
            svs = []
            for e in S.ENGS:
                if S.count[e] > 0:
                    svs.append(S._eng_sem(e, S.count[e]))
            for t in S.dsems:
                svs.append((t.dsem, t.dcount))
            bt = T("barrier")
            for e in S.ENGS:
                waits = []
                for s, v in svs:
                    if e == "pe" and any(s is o for o in S.esems[e]):
                        continue
                    if S.seen[e].get(s, 0) >= v:
                        continue
                    S.seen[e][s] = v
                    waits.append((s, v))
                if waits:
                    S.ops[e].append((waits, None, None))

        def mm_group(out_ap, pairs, reads, writes):
            def fn(e):
                n = len(pairs)
                ins = None
                for i, (l, r) in enumerate(pairs):
                    ins = e.matmul(out_ap, lhsT=l, rhs=r, start=(i == 0), stop=(i == n - 1))
                return ins
            return S.op("pe", fn, reads=reads, writes=writes)

        WA_OFF = 0
        wa = [view(WA_OFF + s * 8192, 8192, BF16).rearrange("p (k n) -> p k n", k=KC) for s in range(2)]
        t_wa = TL(2, "wa")
        S.op("act", lambda e: e.activation(out=cTb[:], in_=small["cT"][:], func=AF.Silu),
             reads=[tsm["cT"]], writes=[t_cTb])
        ada_wv = [dr["w_ada"][i].rearrange("(k p) n -> p k n", p=128) for i in range(2)]

        def ada_dma(i, pc, slot_ap, t_slot, extra_w=()):
            S.dma("pool", lambda e: e.dma_start(out=slot_ap, in_=ada_wv[i][:, :, pc * 512:(pc + 1) * 512]), writes=[t_slot] + list(extra_w))

        def ada_mm(i, pc, slot_ap, t_slot, bank):
            def fn(e):
                ins = None
                for jj in range(4):
                    for k in range(KC):
                        ins = e.matmul(psum[:, bank, jj:jj + 1], lhsT=slot_ap[:, k, jj * 128:(jj + 1) * 128],
                                       rhs=cTb[:, k:k + 1], start=(k == 0), stop=(k == KC - 1))
                return ins
            S.op("pe", fn, reads=[t_slot, t_cTb], writes=[tP[bank]])
            S.op("dve", lambda e: e.tensor_tensor(out=modv[:, i, pc * 4:pc * 4 + 4], in0=psum[:, bank, 0:4],
                                                  in1=small["badaT"][:, i, pc * 4:pc * 4 + 4], op=ALU.add),
                 reads=[tP[bank], tsm["badaT"]], writes=[t_mod])

        for pc in range(4):
            ada_dma(0, pc, wa[pc % 2], t_wa[pc % 2])
            ada_mm(0, pc, wa[pc % 2], t_wa[pc % 2], pc % 2)
        S.op("dve", lambda e: e.scalar_tensor_tensor(out=Amix[:, 0, :], in0=modv[:, 0, 8:16], scalar=1.0, in1=small["gmixT"][:, 0, :],
                                                     op0=ALU.add, op1=ALU.mult), reads=[t_mod, tsm["gmixT"]], writes=[t_A])
        ada_rest = [(0, pc) for pc in range(4, 12)] + [(1, pc) for pc in range(12)]
        lamt = sb("lamt", [128, 2, 64], F32)
        lams = sb("lams", [128, 2], F32)
        lame = sb("lame", [128, 2], F32)
        t_lt = T("lamt")
        S.op("dve", lambda e: e.tensor_tensor(out=lamt[:, 0, :], in0=small["lamv"][:, 0, :], in1=small["lamv"][:, 1, :], op=ALU.mult),
             reads=[tsm["lamv"]], writes=[t_lt])
        S.op("dve", lambda e: e.tensor_tensor(out=lamt[:, 1, :], in0=small["lamv"][:, 2, :], in1=small["lamv"][:, 3, :], op=ALU.mult),
             reads=[tsm["lamv"]], writes=[t_lt])
        S.op("dve", lambda e: e.tensor_reduce(out=lams[:], in_=lamt[:], axis=AX.X, op=ALU.add), reads=[t_lt], writes=[t_lt])
        S.op("act", lambda e: e.activation(out=lame[:], in_=lams[:], func=AF.Exp), reads=[t_lt], writes=[t_lt])
        S.op("dve", lambda e: e.scalar_tensor_tensor(out=neglam[:], in0=lame[:, 1:2], scalar=-LAMBDA_INIT0, in1=lame[:, 0:1],
                                                     op0=ALU.add, op1=ALU.subtract), reads=[t_lt], writes=[t_lam])
        S.op("dve", lambda e: e.tensor_scalar(out=gsubc[:], in0=small["gsub"][:], scalar1=1.0 - LAMBDA_INIT0, scalar2=None, op0=ALU.mult),
             reads=[tsm["gsub"]], writes=[t_gsub])

        R_X, R_H, R_S = 0, 66560, 99840
        xT = view(R_X, 66560, F32).rearrange("p (k n) -> p k n", k=KC)
        t_x = [TL(KC, f"x{ci}_") for ci in range(5)]
        hT = view(R_H, 33280, BF16).rearrange("p (k n) -> p k n", k=KC)
        t_h = TL(5, "h")
        hTp = view(R_X, 32768, BF16).rearrange("p (k n) -> p k n", k=KC)
        t_hp = TL(4, "hp")
        cosT = view(R_X + 32768, 8192, BF16)
        sinT = view(R_X + 40960, 8192, BF16)
        t_cs = T("cossin")
        wq5 = view(R_X + 49152, 5 * 2048, BF16).rearrange("p (w k n) -> p w k n", w=5, k=KC)
        t_w5 = TL(5, "w5")
        eO = [view(R_X + 49152 + 10240 + i * 2048, 2048, F32) for i in range(2)]
        t_eO = TL(2, "eO")
        asq = view(R_X + 49152 + 14336, 1024, BF16)
        t_asq = T("asq")
        ers = view(R_X + 49152 + 15360, 2048, F32)
        t_ers = T("ers")
        wo_sb = view(R_S + 30720, 16384, BF16).rearrange("p (k n) -> p k n", k=KC)
        t_wo = T("wo")
        o = R_S
        xs = view(o, 16384, F32).rearrange("p (k n) -> p k n", k=KC); o += 16384
        sqb = view(o, 8192, BF16).rearrange("p (k n) -> p k n", k=KC); o += 8192
        rstd = view(o, 2048, F32); o += 2048
        tmpA = view(o, 2048, F32); o += 2048
        tmpB = view(o, 2048, F32); o += 2048
        t_xs, t_sqb, t_rstd, t_tmpA, t_tmpB = T("xs"), T("sqb"), T("rstd"), T("tmpA"), T("tmpB")
        qz = view(o, 2 * NT * 2, BF16).rearrange("p (j n) -> p j n", j=2); o += 2 * NT * 2
        kT = view(o, 8192, BF16); o += 8192
        Vc = view(o, 32 * 130 * 2, BF16).rearrange("p (t n) -> p t n", t=32); o += 32 * 130 * 2
        t_q = TL(5, "q"); t_k = TL(8, "k"); t_v = TL(8, "v")
        NPB = 4
        pT = [view(o + i * 1024, 1024, BF16) for i in range(NPB)]; o += NPB * 1024
        t_pT = TL(NPB, "pT")
        oT = view(o, 33280, BF16).rearrange("p (k n) -> p k n", k=KC); o += 33280
        t_oT = TL(5, "oT")
        eL = [view(o, 2048, F32), rstd]; o += 2048
        t_eL = [T("eL0"), t_rstd]
        assert o <= ARENA_W * 4, o

        pos_t = view(R_S + 32768, 16384, I32)
        t_pos = T("pos")

        S.dma("sp", lambda e: e.dma_start(out=pos_t, in_=dr["pos_kv"][0:1, :].to_broadcast([128, NP + NO])), writes=[t_pos])
        TWO_PI = float(2 * np.pi)
        ang = view(R_S, 16384, F32)
        tq = view(R_H, 16384, F32)
        tqi = view(R_H, 16384, I32)
        t_ang, t_tq = T("ang"), T("tq")
        S.op("dve", lambda e: e.tensor_copy(out=tq, in_=pos_t), reads=[t_pos], writes=[t_tq])
        S.op("dve", lambda e: e.tensor_scalar(out=ang, in0=tq, scalar1=small["invf"][:, 0:1], scalar2=None, op0=ALU.mult),
             reads=[t_tq, tsm["invf"]], writes=[t_ang])
        S.op("dve", lambda e: e.tensor_scalar(out=tq, in0=ang, scalar1=1.0 / TWO_PI, scalar2=None, op0=ALU.mult),
             reads=[t_ang], writes=[t_tq])
        S.op("dve", lambda e: e.tensor_copy(out=tqi, in_=tq), reads=[t_tq], writes=[t_tq])
        S.op("dve", lambda e: e.tensor_copy(out=tq, in_=tqi), reads=[t_tq], writes=[t_tq])
        S.op("dve", lambda e: e.scalar_tensor_tensor(out=ang, in0=tq, scalar=-TWO_PI, in1=ang, op0=ALU.mult, op1=ALU.add),
             reads=[t_tq, t_ang], writes=[t_ang])
        S.op("dve", lambda e: e.tensor_scalar(out=tq, in0=ang, scalar1=float(np.pi), scalar2=None, op0=ALU.is_gt), reads=[t_ang], writes=[t_tq])
        S.op("dve", lambda e: e.scalar_tensor_tensor(out=ang, in0=tq, scalar=-TWO_PI, in1=ang, op0=ALU.mult, op1=ALU.add),
             reads=[t_tq, t_ang], writes=[t_ang])
        S.op("dve", lambda e: e.tensor_scalar(out=tq, in0=ang, scalar1=float(-np.pi), scalar2=None, op0=ALU.is_lt), reads=[t_ang], writes=[t_tq])
        S.op("dve", lambda e: e.scalar_tensor_tensor(out=ang, in0=tq, scalar=TWO_PI, in1=ang, op0=ALU.mult, op1=ALU.add),
             reads=[t_tq, t_ang], writes=[t_ang])
        S.op("act", lambda e: e.activation(out=sinT, in_=ang, func=AF.Sin), reads=[t_ang], writes=[t_cs])
        S.op("dve", lambda e: e.scalar_tensor_tensor(out=tq, in0=ang, scalar=-1.0, in1=ang, op0=ALU.mult, op1=ALU.max), reads=[t_ang], writes=[t_tq])
        hpi = sb("hpi", [128, 1], F32)
        t_hpi = T("hpi")
        S.op("pool", lambda e: e.memset(hpi[:], float(np.pi / 2)), writes=[t_hpi])
        S.op("act", lambda e: e.activation(out=cosT, in_=tq, func=AF.Sin, scale=-1.0, bias=hpi[:, 0:1]), reads=[t_tq, t_hpi], writes=[t_cs])
        barrier()

        pb = [1, 2]
        nstat = [0]

        def norm_mod(src_fn, t_src, w, dst_fn, t_dst, A_fn, B_fn, t_AB, dst_is_f32=False, src3d=None):
            bk = pb[nstat[0] % 2]
            nstat[0] += 1
            if src3d is not None:
                S.op("act", lambda e: e.activation(out=sqb[:, :, 0:w], in_=src3d, func=AF.Square), reads=t_src, writes=[t_sqb])
            else:
                for k in range(KC):
                    S.op("act", lambda e, k=k: e.activation(out=sqb[:, k, 0:w], in_=src_fn(k), func=AF.Square),
                         reads=t_src, writes=[t_sqb])
            mm_group(psum[:, bk, 0:w], [(onesD[:], sqb[:, k, 0:w]) for k in range(KC)], [t_ones, t_sqb], [tP[bk]])
            S.op("act", lambda e: e.activation(out=rstd[:, 0:w], in_=psum[:, bk, 0:w], func=AF.Ln, bias=epsc[:, 0:1]), reads=[tP[bk], t_epsc], writes=[t_rstd])
            S.op("act", lambda e: e.activation(out=rstd[:, 0:w], in_=rstd[:, 0:w], func=AF.Exp, scale=-0.5), reads=[t_rstd], writes=[t_rstd])
            for k in range(KC):
                tt, t_tt = (tmpA, t_tmpA) if k % 2 == 0 else (tmpB, t_tmpB)
                S.op("dve", lambda e, k=k, tt=tt: e.tensor_tensor(out=tt[:, 0:w], in0=src_fn(k), in1=rstd[:, 0:w], op=ALU.mult),
                     reads=t_src + [t_rstd], writes=[t_tt])
                if B_fn is None:
                    S.op("act", lambda e, k=k, tt=tt: e.activation(out=dst_fn(k), in_=tt[:, 0:w], func=AF.Copy, scale=A_fn(k)),
                         reads=[t_tt] + t_AB, writes=t_dst)
                else:
                    S.op("act", lambda e, k=k, tt=tt: e.activation(out=dst_fn(k), in_=tt[:, 0:w], func=AF.Identity,
                                                                   scale=A_fn(k), bias=B_fn(k)),
                         reads=[t_tt] + t_AB, writes=t_dst)

        xall_v = dr["xT_all"].rearrange("(k p) n -> p k n", p=128)
        xprev_v = dr["xT_prev"].rearrange("(k p) n -> p k n", p=128)

        def load_xs(src_v, c0, w, buf=None, t_buf=None):
            buf = xs if buf is None else buf
            t_buf = t_xs if t_buf is None else t_buf
            S.dma("sp", lambda e: e.dma_start(out=buf[:, :, 0:w], in_=src_v[:, :, c0:c0 + w]), writes=[t_buf])

        xs2 = view(R_S + 30720 + 2 * NT * 2 + 8192 + 32 * 130 * 2 + NPB * 1024, 16384, F32).rearrange("p (k n) -> p k n", k=KC)
        t_xs2 = T("xs2")
        xbufs = [(xs, t_xs), (xs2, t_xs2)]

        for ci in range(4):
            c0, w = ci * 512, 512
            xb, t_xb = xbufs[ci % 2]
            load_xs(xprev_v, c0, w, xb, t_xb)
            norm_mod(lambda k, xb=xb: xb[:, k, 0:w], [t_xb], w, lambda k, c0=c0, w=w: hTp[:, k, c0:c0 + w], [t_hp[ci]],
                     lambda k: Amix[:, 0, k:k + 1], lambda k: modv[:, 0, k:k + 1], [t_A, t_mod], src3d=xb[:, :, 0:w])
        for ci, (c0, w) in enumerate(CH):
            xb, t_xb = xbufs[ci % 2]
            load_xs(xall_v, c0, w, xb, t_xb)
            norm_mod(lambda k, xb=xb: xb[:, k, 0:w], [t_xb], w, lambda k, c0=c0, w=w: hT[:, k, c0:c0 + w], [t_h[ci]],
                     lambda k: Amix[:, 0, k:k + 1], lambda k: modv[:, 0, k:k + 1], [t_A, t_mod], src3d=xb[:, :, 0:w])

        wqkv = dr["attn_w_qkv"].rearrange("(k p) n -> p k n", p=128)

        def h_kv(ch, k, a=0, wd=512):
            if ch < 4:
                return hTp[:, k, ch * 512 + a: ch * 512 + a + wd]
            return hT[:, k, (ch - 4) * 512 + a:(ch - 4) * 512 + a + wd]

        def t_hkv(ch):
            return t_hp[ch] if ch < 4 else t_h[ch - 4]

        S.op("pool", lambda e: e.memset(qz[:, :, :], 0.0), writes=t_q)
        pT_i = [0]
        sbank_i = [0]
        gstep = [0]
        rp_i = [0]
        ada_t = [0]
        wa2 = [view(R_S + sl * 8192, 8192, BF16).rearrange("p (k n) -> p k n", k=KC) for sl in range(2)]
        t_wa2 = TL(2, "wa2")

        def ada_task():
            t = ada_t[0]
            if t >= len(ada_rest) + 2:
                return
            ada_t[0] += 1
            if 2 <= t:
                i, pc = ada_rest[t - 2]
                ada_mm(i, pc, wa2[t % 2], t_wa2[t % 2], 3)
            if t < len(ada_rest):
                i, pc = ada_rest[t]
                ada_dma(i, pc, wa2[t % 2], t_wa2[t % 2], extra_w=[t_xs])
        epb_i = [0]
        SB3 = [0, 4, 5]
        deferred = []
        for c in range(KC):
            for wi, col in ((0, c * 128), (2, D + c * 128), (4, 2 * D + c * 128)):
                S.dma("pool", lambda e, wi=wi, col=col: e.dma_start(out=wq5[:, wi], in_=wqkv[:, :, col:col + 128]), writes=[t_w5[wi]])
            for wi in (0, 2):
                for (dst, src, sgn) in ((0, 32, -1.0), (32, 0, 1.0), (64, 96, -1.0), (96, 64, 1.0)):
                    S.op("pool", lambda e, wi=wi, dst=dst, src=src, sgn=sgn: e.tensor_scalar(
                        out=wq5[:, wi + 1, :, dst:dst + 32], in0=wq5[:, wi, :, src:src + 32], scalar1=sgn, scalar2=None, op0=ALU.mult),
                        reads=[t_w5[wi]], writes=[t_w5[wi + 1]])

            def rope_proj(wi, rhs_fn, t_rhs, w, tab0, dst_ap, t_dst, qcol=0):
                ba, bb = (3, 4) if rp_i[0] % 2 == 0 else (0, 1)
                rp_i[0] += 1
                mm_group(psum[:, ba, 0:w], [(wq5[:, wi, k, :], rhs_fn(k)) for k in range(KC)], [t_w5[wi]] + t_rhs, [tP[ba]])
                mm_group(psum[:, bb, 0:w], [(wq5[:, wi + 1, k, :], rhs_fn(k)) for k in range(KC)], [t_w5[wi + 1]] + t_rhs, [tP[bb]])
                S.op("dve", lambda e: e.tensor_tensor(out=tmpA[:, 0:w], in0=psum[:, ba, 0:w], in1=cosT[:, tab0:tab0 + w], op=ALU.mult),
                     reads=[tP[ba], t_cs], writes=[t_tmpA])
                S.op("dve", lambda e: e.tensor_tensor(out=tmpB[:, 0:w], in0=psum[:, bb, 0:w], in1=sinT[:, tab0:tab0 + w], op=ALU.mult),
                     reads=[tP[bb], t_cs], writes=[t_tmpB])
                if dst_ap is None:
                    for jq in range(2):
                        S.op("pool", lambda e, jq=jq: e.tensor_tensor(out=qz[64 * jq:64 * jq + 64, jq, qcol:qcol + w], in0=tmpA[64 * jq:64 * jq + 64, 0:w],
                                                                  in1=tmpB[64 * jq:64 * jq + 64, 0:w], op=ALU.add),
                             reads=[t_tmpA, t_tmpB], writes=t_dst)
                else:
                    S.op("pool", lambda e: e.tensor_tensor(out=dst_ap, in0=tmpA[:, 0:w], in1=tmpB[:, 0:w], op=ALU.add),
                         reads=[t_tmpA, t_tmpB], writes=t_dst)

            for ch in range(8):
                rope_proj(2, lambda k, ch=ch: h_kv(ch, k), [t_hkv(ch)], 512, ch * 512, kT[:, ch * 512:(ch + 1) * 512], [t_k[ch]])
            for ci, (c0, w) in enumerate(CH):
                tab0 = NP + c0 if ci < 4 else NP - HALO
                rope_proj(0, lambda k, c0=c0, w=w: hT[:, k, c0:c0 + w], [t_h[ci]], w, tab0, None, [t_q[ci]], qcol=c0)
            for ch in range(8):
                for tt in range(4):
                    mm_group(psum[:, 5, tt * 128:(tt + 1) * 128],
                             [(h_kv(ch, k, tt * 128, 128), wq5[:, 4, k, :]) for k in range(KC)], [t_w5[4], t_hkv(ch)], [tP[5]])
                S.op("act", lambda e, ch=ch: e.activation(out=Vc[:, ch * 4:(ch + 1) * 4, 0:128],
                                                          in_=psum[:, 5, :].rearrange("p (t n) -> p t n", t=4), func=AF.Copy),
                     reads=[tP[5]], writes=[t_v[ch]])

            steps = []
            for gi, (c0, w) in enumerate(CH):
                if gi < 4:
                    ktiles = [(kt, "prev") for kt in range(16)] + [(16 + kt, "full") for kt in range(4 * gi)] + \
                             [(16 + 4 * gi + t, ("diag", t)) for t in range(4)]
                    nqs = 4
                else:
                    ktiles = [(kt, "prev") for kt in range(15)] + [(15, "hdiag")]
                    nqs = 1
                for j in range(2):
                    for ki, (kt, kind) in enumerate(ktiles):
                        steps.append(dict(gi=gi, c0=c0, w=w, j=j, ki=ki, kt=kt, kind=kind, last=(ki == len(ktiles) - 1),
                                          nqs=nqs, qw=min(w, 128)))

            def emit_qk(sp):
                w, c0, j, kt, kind, gi = sp["w"], sp["c0"], sp["j"], sp["kt"], sp["kind"], sp["gi"]
                pj = slice(64 * j, 64 * j + 64)
                sbk = SB3[sbank_i[0] % 3]
                sbank_i[0] += 1
                pairs = [(kT[:, kt * 128:(kt + 1) * 128], qz[:, j, c0:c0 + w])]
                rds = [t_k[kt // 4], t_q[gi]]
                if isinstance(kind, tuple):
                    pairs.append((small["ident_b"][:], small["masks"][:, kind[1], :]))
                    rds += [tsm["ident_b"], tsm["masks"]]
                elif kind == "hdiag":
                    pairs.append((small["ident_b"][:], small["maskh"][:]))
                    rds += [tsm["ident_b"], tsm["maskh"]]
                mm_group(psum[:, sbk, 0:w], pairs, rds, [tP[sbk]])
                pi = pT_i[0] % NPB
                pT_i[0] += 1
                sp["pi"] = pi
                if kind in ("prev", "hdiag"):
                    S.op("act", lambda e: e.activation(out=pT[pi][:, 0:w], in_=psum[:, sbk, 0:w], func=AF.Exp,
                                                       scale=0.125, bias=small["visb"][:, 0:1]),
                         reads=[tP[sbk], tsm["visb"]], writes=[t_pT[pi]])
                else:
                    S.op("act", lambda e: e.activation(out=pT[pi][:, 0:w], in_=psum[:, sbk, 0:w], func=AF.Exp, scale=0.125),
                         reads=[tP[sbk]], writes=[t_pT[pi]])

            def emit_pv(sp):
                j, kt, ki, last, w, pi = sp["j"], sp["kt"], sp["ki"], sp["last"], sp["w"], sp["pi"]
                obk = 6 + j
                lbk = 1 + j

                def pv(e):
                    e.matmul(psum[:, obk, 0:w], lhsT=Vc[:, kt, 0:128], rhs=pT[pi][:, 0:w], start=(ki == 0), stop=last)
                    return e.matmul(psum[:, lbk, 0:w], lhsT=small["ident_b"][:], rhs=pT[pi][:, 0:w], start=(ki == 0), stop=last)
                S.op("pe", pv, reads=[t_pT[pi], t_v[kt // 4], tsm["ident_b"]], writes=[tP[obk], tP[lbk]])
                if last:
                    while deferred:
                        deferred.pop(0)[1]()
                    S.op("dve", lambda e: e.tensor_copy(out=eO[j][:, 0:w], in_=psum[:, obk, 0:w]),
                         reads=[tP[obk]], writes=[t_eO[j]])
                    S.op("dve", lambda e: e.tensor_scalar(out=eL[j][:, 0:w], in0=psum[:, lbk, 0:w], scalar1=1e-32, scalar2=None, op0=ALU.add),
                         reads=[tP[lbk]], writes=[t_eL[j]])

            def epilogue_tasks(sp, s_end):
                gi, c0, w = sp["gi"], sp["c0"], sp["w"]

                def tA():
                    mm_group(psum[:, 3, 0:w], [(onesF1[:], eL[0][:, 0:w])], [t_ones, t_eL[0]], [tP[3]])
                    S.op("dve", lambda e: e.reciprocal(out=eL[0][:, 0:w], in_=psum[:, 3, 0:w]), reads=[tP[3]], writes=[t_eL[0]])
                    S.op("dve", lambda e: e.tensor_tensor(out=eO[0][:, 0:w], in0=eO[0][:, 0:w], in1=eL[0][:, 0:w], op=ALU.mult),
                         reads=[t_eO[0], t_eL[0]], writes=[t_eO[0]])

                def tB():
                    mm_group(psum[:, 3, 0:w], [(onesF1[:], eL[1][:, 0:w])], [t_ones, t_eL[1]], [tP[3]])
                    S.op("dve", lambda e: e.reciprocal(out=eL[1][:, 0:w], in_=psum[:, 3, 0:w]), reads=[tP[3]], writes=[t_eL[1]])
                    S.op("dve", lambda e: e.scalar_tensor_tensor(out=eO[1][:, 0:w], in0=eO[1][:, 0:w], scalar=neglam[:, 0:1], in1=eL[1][:, 0:w],
                                                                 op0=ALU.mult, op1=ALU.mult), reads=[t_eO[1], t_eL[1], t_lam], writes=[t_eO[1]])
                    S.op("dve", lambda e: e.tensor_tensor(out=eO[0][:, 0:w], in0=eO[0][:, 0:w], in1=eO[1][:, 0:w], op=ALU.add),
                         reads=[t_eO[0], t_eO[1]], writes=[t_eO[0]])
                    S.op("dve", lambda e: e.tensor_tensor(out=asq[:, 0:w], in0=eO[0][:, 0:w], in1=eO[0][:, 0:w], op=ALU.mult), reads=[t_eO[0]], writes=[t_asq])

                def tC():
                    mm_group(psum[:, 3, 0:w], [(ones128[:], asq[:, 0:w])], [t_ones, t_asq], [tP[3]])
                    S.op("act", lambda e: e.activation(out=ers[:, 0:w], in_=psum[:, 3, 0:w], func=AF.Ln, bias=epsc[:, 0:1]),
                         reads=[tP[3], t_epsc], writes=[t_ers])
                    S.op("act", lambda e: e.activation(out=ers[:, 0:w], in_=ers[:, 0:w], func=AF.Exp, scale=-0.5), reads=[t_ers], writes=[t_ers])
                    S.op("dve", lambda e: e.scalar_tensor_tensor(out=oT[:, c, c0:c0 + w], in0=eO[0][:, 0:w], scalar=gsubc[:, 0:1], in1=ers[:, 0:w],
                                                                 op0=ALU.mult, op1=ALU.mult), reads=[t_eO[0], t_ers, t_gsub], writes=[t_oT[gi]])
                return [(s_end + 2, tA), (s_end + 9, tB), (s_end + 18, tC)]

            LOOK = 2
            for idx in range(len(steps) + LOOK):
                while deferred and deferred[0][0] <= idx:
                    deferred.pop(0)[1]()
                gstep[0] += 1
                if gstep[0] % 60 == 30:
                    ada_task()
                if idx < len(steps):
                    emit_qk(steps[idx])
                if idx >= LOOK:
                    sp = steps[idx - LOOK]
                    emit_pv(sp)
                    if sp["last"] and sp["j"] == 1:
                        deferred.extend(epilogue_tasks(sp, idx))
                        deferred.sort(key=lambda t: t[0])
            for _, fl in deferred:
                fl()
            deferred.clear()

        while ada_t[0] < len(ada_rest) + 2:
            ada_task()
        S.op("dve", lambda e: e.scalar_tensor_tensor(out=Affn[:], in0=modv[:, :, 32:40], scalar=1.0, in1=small["gffnT"][:],
                                                     op0=ALU.add, op1=ALU.mult), reads=[t_mod, tsm["gffnT"]], writes=[t_A])
        S.op("dve", lambda e: e.scalar_tensor_tensor(out=Amix[:, 1, :], in0=modv[:, 1, 8:16], scalar=1.0, in1=small["gmixT"][:, 1, :],
                                                     op0=ALU.add, op1=ALU.mult), reads=[t_mod, tsm["gmixT"]], writes=[t_A])
        yb = [4, 5, 6, 7]
        yb_i = [0]

        def proj_residual(w_sb, t_w, rhs_fn, t_rhs, ci, c0, w, Gcol_fn, t_G, src_fn, t_src, bias_fn=None, t_bias=()):
            for d in range(KC):
                bk = yb[yb_i[0] % 4]
                yb_i[0] += 1
                mm_group(psum[:, bk, 0:w], [(w_sb[:, k, d * 128:(d + 1) * 128], rhs_fn(k)) for k in range(KC)], [t_w] + t_rhs, [tP[bk]])
                if bias_fn is None:
                    S.op("dve", lambda e, d=d, bk=bk: e.scalar_tensor_tensor(out=xT[:, d, c0:c0 + w], in0=psum[:, bk, 0:w], scalar=Gcol_fn(d),
                                                                             in1=src_fn(d), op0=ALU.mult, op1=ALU.add),
                         reads=[tP[bk]] + t_G + t_src(d), writes=[t_x[ci][d]])
                else:
                    S.op("act", lambda e, d=d, bk=bk: e.activation(out=tmpA[:, 0:w], in_=psum[:, bk, 0:w], func=AF.Identity, bias=bias_fn(d)),
                         reads=[tP[bk]] + list(t_bias), writes=[t_tmpA])
                    S.op("dve", lambda e, d=d: e.scalar_tensor_tensor(out=xT[:, d, c0:c0 + w], in0=tmpA[:, 0:w], scalar=Gcol_fn(d),
                                                                      in1=src_fn(d), op0=ALU.mult, op1=ALU.add),
                         reads=[t_tmpA] + t_G + t_src(d), writes=[t_x[ci][d]])

        barrier()
        S.dma("pool", lambda e: e.dma_start(out=wo_sb, in_=dr["attn_w_o"].rearrange("(k p) n -> p k n", p=128)), writes=[t_wo])
        for ci, (c0, w) in enumerate(CH):
            load_xs(xall_v, c0, w)
            proj_residual(wo_sb, t_wo, lambda k, c0=c0, w=w: oT[:, k, c0:c0 + w], [t_oT[ci]], ci, c0, w,
                          lambda d: modv[:, 0, 16 + d:17 + d], [t_mod], lambda d, w=w: xs[:, d, 0:w], lambda d: [t_xs])
            if stage > 1:
                norm_mod(lambda k, c0=c0, w=w: xT[:, k, c0:c0 + w], t_x[ci], w, lambda k, c0=c0, w=w: hT[:, k, c0:c0 + w], [t_h[ci]],
                         lambda k: Affn[:, 0, k:k + 1], lambda k: modv[:, 0, 24 + k:25 + k], [t_A, t_mod], src3d=xT[:, :, c0:c0 + w])
        if stage <= 1:
            return finish(nc, S, st, outT, xT, t_x)

        barrier()
        o = R_S + 16384 + 8192 + 3 * 2048
        FGW = 512
        wg_sb = [view(o + s * 8192, 8192, BF16).rearrange("p (k n) -> p k n", k=KC) for s in range(2)]; o += 16384
        wu_sb = [view(o + s * 8192, 8192, BF16).rearrange("p (k n) -> p k n", k=KC) for s in range(2)]; o += 16384
        wd_one = view(o, 8192, BF16).rearrange("p (c n) -> p c n", c=4); o += 8192
        wd_sb = [wd_one, wd_one]
        t_wd1 = T("wd")
        t_wg, t_wu, t_wd = TL(2, "wg"), TL(2, "wu"), [t_wd1, t_wd1]
        sg = [view(o + i * 1024, 1024, BF16) for i in range(2)]; o += 2048
        t_sg = TL(2, "sg")
        actb = [view(o + i * 4096, 4096, BF16).rearrange("p (c n) -> p c n", c=4) for i in range(2)]; o += 8192
        t_act = TL(2, "act")
        FFN_END = o
        assert o <= ARENA_W * 4, o
        ffn_i = [0]
        gu_i = [0]
        act_i = [0]

        def ffn(wg_d, wu_d, wd_d, F, chunks, hg_fn, t_hg, hu_fn, t_hu, G_fn, t_G, tok_gate=None, on_chunk_done=None, first_gu_preloaded=False):
            wgv = wg_d.rearrange("(k p) f -> p k f", p=128)
            wuv = wu_d.rearrange("(k p) f -> p k f", p=128)
            nfg = (F + FGW - 1) // FGW

            def down(sl, nfc, ai, ci, c0, w, is_last_fg):
                for d in range(KC):
                    bk = yb[yb_i[0] % 4]
                    yb_i[0] += 1
                    mm_group(psum[:, bk, 0:w], [(wd_sb[sl][:, fc, d * 128:(d + 1) * 128], actb[ai][:, fc, 0:w]) for fc in range(nfc)],
                             [t_wd[sl], t_act[ai]], [tP[bk]])
                    S.op("dve", lambda e, d=d, bk=bk: e.scalar_tensor_tensor(
                        out=xT[:, d, c0:c0 + w], in0=psum[:, bk, 0:w], scalar=G_fn(d), in1=xT[:, d, c0:c0 + w], op0=ALU.mult, op1=ALU.add),
                        reads=[tP[bk]] + t_G + [t_x[ci][d]], writes=[t_x[ci][d]])
                if is_last_fg and on_chunk_done is not None:
                    on_chunk_done(ci, c0, w)

            pend = None
            for fg in range(nfg):
                f0 = fg * FGW
                fw = min(FGW, F - f0)
                nfc = fw // 128
                sl = ffn_i[0] % 2
                ffn_i[0] += 1
                if not (first_gu_preloaded and fg == 0):
                    S.dma("pool", lambda e, sl=sl, f0=f0, fw=fw: e.dma_start(out=wg_sb[sl][:, :, 0:fw], in_=wgv[:, :, f0:f0 + fw]), writes=[t_wg[sl]])
                    S.dma("pool", lambda e, sl=sl, f0=f0, fw=fw: e.dma_start(out=wu_sb[sl][:, :, 0:fw], in_=wuv[:, :, f0:f0 + fw]), writes=[t_wu[sl]])
                first = True
                for (ci, c0, w) in chunks:
                    ai = act_i[0] % 2
                    act_i[0] += 1
                    for fc in range(nfc):
                        gi_ = gu_i[0] % 2
                        gu_i[0] += 1
                        bg, bu = (0, 2) if gi_ == 0 else (1, 3)
                        mm_group(psum[:, bg, 0:w], [(wg_sb[sl][:, k, fc * 128:(fc + 1) * 128], hg_fn(k, c0, w)) for k in range(KC)],
                                 [t_wg[sl]] + t_hg(ci), [tP[bg]])
                        mm_group(psum[:, bu, 0:w], [(wu_sb[sl][:, k, fc * 128:(fc + 1) * 128], hu_fn(k, c0, w)) for k in range(KC)],
                                 [t_wu[sl]] + t_hu(ci), [tP[bu]])
                        S.op("act", lambda e, gi_=gi_, bg=bg: e.activation(out=sg[gi_][:, 0:w], in_=psum[:, bg, 0:w], func=AF.Silu),
                             reads=[tP[bg]], writes=[t_sg[gi_]])
                        S.op("dve", lambda e, gi_=gi_, bu=bu, ai=ai, fc=fc: e.tensor_tensor(out=actb[ai][:, fc, 0:w], in0=psum[:, bu, 0:w],
                                                                                          in1=sg[gi_][:, 0:w], op=ALU.mult),
                             reads=[tP[bu], t_sg[gi_]], writes=[t_act[ai]])
                        if tok_gate is not None:
                            S.op("dve", lambda e, ai=ai, fc=fc, ci=ci, w=w: e.tensor_tensor(out=actb[ai][:, fc, 0:w], in0=actb[ai][:, fc, 0:w],
                                                                                          in1=tok_gate[0](ci, w), op=ALU.mult),
                                 reads=[t_act[ai]] + tok_gate[1](ci), writes=[t_act[ai]])
                    if pend is not None:
                        down(*pend)
                    if first:
                        S.dma("pool", lambda e, sl=sl, f0=f0, fw=fw, nfc=nfc: e.dma_start(
                            out=wd_sb[sl][:, 0:nfc, :], in_=wd_d[f0:f0 + fw, :].rearrange("(c p) d -> p c d", p=128)), writes=[t_wd[sl]])
                        first = False
                    pend = (sl, nfc, ai, ci, c0, w, fg == nfg - 1)
            if pend is not None:
                down(*pend)

        ffn(dr["ffn_w_gate"], dr["ffn_w_up"], dr["ffn_w_down"], FF0, [(ci, c0, w) for ci, (c0, w) in enumerate(CH)],
            lambda k, c0, w: hT[:, k, c0:c0 + w], lambda ci: [t_h[ci]], lambda k, c0, w: hT[:, k, c0:c0 + w], lambda ci: [t_h[ci]],
            lambda d: modv[:, 0, 40 + d:41 + d], [t_mod],
            on_chunk_done=(None if stage <= 2 else (lambda ci, c0, w: norm_mod(
                lambda k: xT[:, k, c0:c0 + w], t_x[ci], w, lambda k: hT[:, k, c0:c0 + w], [t_h[ci]],
                lambda k: Amix[:, 1, k:k + 1], lambda k: modv[:, 1, k:k + 1], [t_A, t_mod], src3d=xT[:, :, c0:c0 + w]))))
        if stage <= 2:
            return finish(nc, S, st, outT, xT, t_x)

        barrier()
        o = R_S + 16384 + 8192 + 3 * 2048
        UW = HALO + NO
        uT = view(o, KC * UW * 2, BF16).rearrange("p (k n) -> p k n", k=KC); o += KC * UW * 2
        t_u = TL(KC, "u")
        wpw2 = [view(o + i * 4096, 4096, BF16).rearrange("p (k n) -> p k n", k=KC) for i in range(2)]; o += 8192
        t_wpw2 = TL(2, "wpw")
        dg = [view(o + i * 7936, 7936, BF16).rearrange("p (j n) -> p j n", j=31) for i in range(2)]; o += 2 * 7936
        t_dg = TL(2, "dg")
        sgl = view(o, 2048, F32); o += 2048
        t_sgl = T("sgl")
        lnm = view(o, 2048, F32); o += 2048
        lnr = view(o, 2048, F32); o += 2048
        t_lnm, t_lnr = T("lnm"), T("lnr")
        assert o <= ARENA_W * 4, o
        vT = xs
        t_vT = TL(KC, "v")
        vsq = sqb
        t_vsq = t_sqb
        zT = sqb
        t_z = t_sqb
        pw1v = dr["conv_w_pw1"].rearrange("(k p) n -> p k n", p=128)

        for fcn in range(KC):
            wpw, t_wpw = wpw2[fcn % 2], t_wpw2[fcn % 2]
            S.dma("pool", lambda e, fcn=fcn: e.dma_start(out=wpw[:, :, 0:128], in_=pw1v[:, :, fcn * 128:(fcn + 1) * 128]), writes=[t_wpw])
            S.dma("pool", lambda e, fcn=fcn: e.dma_start(out=wpw[:, :, 128:256], in_=pw1v[:, :, D + fcn * 128:D + (fcn + 1) * 128]), writes=[t_wpw])
            for ci, (c0, w) in enumerate(CH):
                pa, pg = (0, 1) if (ci % 2 == 0) else (2, 3)
                mm_group(psum[:, pa, 0:w], [(wpw[:, k, 0:128], hT[:, k, c0:c0 + w]) for k in range(KC)], [t_wpw, t_h[ci]], [tP[pa]])
                mm_group(psum[:, pg, 0:w], [(wpw[:, k, 128:256], hT[:, k, c0:c0 + w]) for k in range(KC)], [t_wpw, t_h[ci]], [tP[pg]])
                S.op("act", lambda e, fcn=fcn, w=w: e.activation(out=sgl[:, 0:w], in_=psum[:, pg, 0:w], func=AF.Sigmoid,
                                                               bias=small["bpw1T"][:, 8 + fcn:9 + fcn]), reads=[tP[pg], tsm["bpw1T"]], writes=[t_sgl])
                if ci < 4:
                    u0 = HALO + c0
                    S.op("dve", lambda e, fcn=fcn, w=w, u0=u0: e.scalar_tensor_tensor(
                        out=uT[:, fcn, u0:u0 + w], in0=psum[:, pa, 0:w], scalar=small["bpw1T"][:, fcn:fcn + 1], in1=sgl[:, 0:w],
                        op0=ALU.add, op1=ALU.mult), reads=[tP[pa], tsm["bpw1T"], t_sgl], writes=[t_u[fcn]])
                else:
                    S.op("dve", lambda e, fcn=fcn, w=w: e.scalar_tensor_tensor(
                        out=tmpA[:, 0:w], in0=psum[:, pa, 0:w], scalar=small["bpw1T"][:, fcn:fcn + 1], in1=sgl[:, 0:w],
                        op0=ALU.add, op1=ALU.mult), reads=[tP[pa], tsm["bpw1T"], t_sgl], writes=[t_tmpA])
                    S.op("dve", lambda e, fcn=fcn, w=w: e.tensor_scalar(out=uT[:, fcn, 0:w], in0=tmpA[:, 0:w], scalar1=small["hflag"][:, 0:1],
                                                                      scalar2=None, op0=ALU.mult), reads=[t_tmpA, tsm["hflag"]], writes=[t_u[fcn]])
        w2_sb = view(R_H, 16384, BF16).rearrange("p (k n) -> p k n", k=KC)
        t_w2 = T("w2")
        barrier()
        S.dma("pool", lambda e: e.dma_start(out=w2_sb, in_=dr["conv_w_pw2"].rearrange("(k p) n -> p k n", p=128)), writes=[t_w2])
        dg_i = [0]
        for ci, (c0, w) in enumerate(CHO):
            for fcn in range(KC):
                di = dg_i[0] % 2
                dg_i[0] += 1
                S.op("dve", lambda e, fcn=fcn, di=di: e.tensor_tensor(
                    out=dg[di][:, :, :], in0=small["ident_b"][:].rearrange("p (o n) -> p o n", o=1).to_broadcast([128, 31, 128]),
                    in1=small["wdwT"][:, fcn, :].rearrange("p (j o) -> p j o", o=1).to_broadcast([128, 31, 128]), op=ALU.mult),
                    reads=[tsm["ident_b"], tsm["wdwT"]], writes=[t_dg[di]])
                bk = fcn % 2
                mm_group(psum[:, bk, 0:w], [(dg[di][:, j, :], uT[:, fcn, c0 + 2 + j:c0 + 2 + j + w]) for j in range(31)],
                         [t_dg[di], t_u[fcn]], [tP[bk]])
                S.op("act", lambda e, fcn=fcn, bk=bk, w=w: e.activation(out=vT[:, fcn, 0:w], in_=psum[:, bk, 0:w], func=AF.Identity,
                                                                      bias=small["bdwT"][:, fcn:fcn + 1]), reads=[tP[bk], tsm["bdwT"]], writes=[t_vT[fcn]])
                S.op("act", lambda e, fcn=fcn, w=w: e.activation(out=vsq[:, fcn, 0:w], in_=vT[:, fcn, 0:w], func=AF.Square),
                     reads=[t_vT[fcn]], writes=[t_vsq])
            mm_group(psum[:, 2, 0:w], [(onesF[:], vT[:, k, 0:w]) for k in range(KC)], [t_ones] + t_vT, [tP[2]])
            mm_group(psum[:, 3, 0:w], [(onesD[:], vsq[:, k, 0:w]) for k in range(KC)], [t_ones, t_vsq], [tP[3]])
            S.op("act", lambda e, w=w: e.activation(out=lnm[:, 0:w], in_=psum[:, 2, 0:w], func=AF.Copy), reads=[tP[2]], writes=[t_lnm])
            S.op("dve", lambda e, w=w: e.tensor_tensor(out=lnr[:, 0:w], in0=lnm[:, 0:w], in1=lnm[:, 0:w], op=ALU.mult), reads=[t_lnm], writes=[t_lnr])
            S.op("dve", lambda e, w=w: e.tensor_tensor(out=lnr[:, 0:w], in0=psum[:, 3, 0:w], in1=lnr[:, 0:w], op=ALU.subtract), reads=[tP[3], t_lnr], writes=[t_lnr])
            S.op("act", lambda e, w=w: e.activation(out=lnr[:, 0:w], in_=lnr[:, 0:w], func=AF.Ln, bias=epsc[:, 0:1]), reads=[t_lnr, t_epsc], writes=[t_lnr])
            S.op("act", lambda e, w=w: e.activation(out=lnr[:, 0:w], in_=lnr[:, 0:w], func=AF.Exp, scale=-0.5), reads=[t_lnr], writes=[t_lnr])
            for fcn in range(KC):
                S.op("dve", lambda e, fcn=fcn, w=w: e.tensor_tensor(out=vT[:, fcn, 0:w], in0=vT[:, fcn, 0:w], in1=lnm[:, 0:w], op=ALU.subtract),
                     reads=[t_vT[fcn], t_lnm], writes=[t_vT[fcn]])
                S.op("dve", lambda e, fcn=fcn, w=w: e.tensor_tensor(out=vT[:, fcn, 0:w], in0=vT[:, fcn, 0:w], in1=lnr[:, 0:w], op=ALU.mult),
                     reads=[t_vT[fcn], t_lnr], writes=[t_vT[fcn]])
                S.op("act", lambda e, fcn=fcn, w=w: e.activation(out=zT[:, fcn, 0:w], in_=vT[:, fcn, 0:w], func=AF.Silu,
                                                               scale=small["lngT"][:, fcn:fcn + 1], bias=small["lnbT"][:, fcn:fcn + 1]),
                     reads=[t_vT[fcn], tsm["lngT"], tsm["lnbT"]], writes=[t_z])
            proj_residual(w2_sb, t_w2, lambda k, w=w: zT[:, k, 0:w], [t_z], ci, c0, w,
                          lambda d: modv[:, 1, 16 + d:17 + d], [t_mod], lambda d, c0=c0, w=w: xT[:, d, c0:c0 + w], lambda d, ci=ci: [t_x[ci][d]],
                          bias_fn=lambda d: small["bpw2T"][:, d:d + 1], t_bias=[tsm["bpw2T"]])
        if stage <= 3:
            return finish(nc, S, st, outT, xT, t_x)

        barrier()
        o = FFN_END
        hf = xs
        t_hf = t_xs
        gT = view(o, NO * 4, F32); o += NO * 4
        t_gT = T("gT")
        Gb = view(o, 4096, BF16).rearrange("p (c n) -> p c n", c=4); o += 4096
        t_Gb = TL(4, "Gb")
        wr = view(o, 256, F32).rearrange("p (k n) -> p k n", k=KC); o += 256
        t_wr = T("wr")
        assert o <= ARENA_W * 4, o
        S.dma("sp", lambda e: e.dma_start(out=wr, in_=dr["moe_w_router"].rearrange("(k p) n -> p k n", p=128)), writes=[t_wr])
        sl0 = ffn_i[0] % 2
        S.dma("pool", lambda e: e.dma_start(out=wg_sb[sl0][:, :, 0:FGW], in_=dr["moe_w_gate"][0].rearrange("(k p) f -> p k f", p=128)[:, :, 0:FGW]), writes=[t_wg[sl0]])
        S.dma("pool", lambda e: e.dma_start(out=wu_sb[sl0][:, :, 0:FGW], in_=dr["moe_w_up"][0].rearrange("(k p) f -> p k f", p=128)[:, :, 0:FGW]), writes=[t_wu[sl0]])
        for ci, (c0, w) in enumerate(CHO):
            norm_mod(lambda k, c0=c0, w=w: xT[:, k, c0:c0 + w], t_x[ci], w, lambda k, w=w: hf[:, k, 0:w], [t_hf],
                     lambda k: Affn[:, 1, k:k + 1], lambda k: modv[:, 1, 24 + k:25 + k], [t_A, t_mod], src3d=xT[:, :, c0:c0 + w])
            S.op("dve", lambda e, c0=c0, w=w: e.tensor_copy(out=hT[:, :, c0:c0 + w], in_=hf[:, :, 0:w]), reads=[t_hf], writes=[t_h[ci]])
            for tt in range(4):
                t16 = ci * 4 + tt
                mm_group(psum[:, 0, t16 * 8:(t16 + 1) * 8], [(hf[:, k, tt * 128:(tt + 1) * 128], wr[:, k, :]) for k in range(KC)],
                         [t_hf, t_wr], [tP[0]])
        rsc = view(R_S + 73728, 4096, F32)
        t_rsc = T("rsc")
        Lf, EQf, L2f, Ef, Gf = (rsc[:, i * 128:(i + 1) * 128] for i in range(5))
        smx = rsc[:, 640:704]
        r3 = lambda ap: ap.rearrange("p (t e) -> p t e", e=8)
        bc = lambda ap: ap.rearrange("p (t o) -> p t o", o=1).to_broadcast([128, 16, 8])
        l1, l2, den, rden = smx[:, 0:16], smx[:, 16:32], smx[:, 32:48], smx[:, 48:64]
        RS = dict(reads=[t_rsc], writes=[t_rsc])
        S.op("dve", lambda e: e.tensor_copy(out=Lf, in_=psum[:, 0, 0:128]), reads=[tP[0]], writes=[t_rsc])
        S.op("dve", lambda e: e.tensor_reduce(out=l1, in_=r3(Lf), axis=AX.X, op=ALU.max), **RS)
        S.op("dve", lambda e: e.tensor_tensor(out=r3(EQf), in0=r3(Lf), in1=bc(l1), op=ALU.is_equal), **RS)
        S.op("dve", lambda e: e.scalar_tensor_tensor(out=L2f, in0=EQf, scalar=-1e30, in1=Lf, op0=ALU.mult, op1=ALU.add), **RS)
        S.op("dve", lambda e: e.tensor_reduce(out=l2, in_=r3(L2f), axis=AX.X, op=ALU.max), **RS)
        S.op("dve", lambda e: e.tensor_tensor(out=r3(EQf), in0=r3(Lf), in1=bc(l2), op=ALU.is_ge), **RS)
        S.op("dve", lambda e: e.tensor_tensor(out=r3(L2f), in0=r3(Lf), in1=bc(l1), op=ALU.subtract), **RS)
        S.op("act", lambda e: e.activation(out=Ef, in_=L2f, func=AF.Exp), **RS)
        S.op("dve", lambda e: e.tensor_tensor(out=Ef, in0=Ef, in1=EQf, op=ALU.mult), **RS)
        S.op("dve", lambda e: e.tensor_reduce(out=den, in_=r3(Ef), axis=AX.X, op=ALU.add), **RS)
        S.op("dve", lambda e: e.reciprocal(out=rden, in_=den), **RS)
        S.op("dve", lambda e: e.tensor_tensor(out=r3(Gf), in0=r3(Ef), in1=bc(rden), op=ALU.mult), **RS)
        for ci in range(4):
            def trs(e, ci=ci):
                ins = None
                for tt in range(4):
                    t16 = ci * 4 + tt
                    ins = e.transpose(out=psum[0:8, 4 + ci, tt * 128:(tt + 1) * 128], in_=Gf[:, t16 * 8:(t16 + 1) * 8], identity=small["ident_f"][:])
                return ins
            S.op("pe", trs, reads=[t_rsc, tsm["ident_f"]], writes=[tP[4 + ci]])
            S.op("dve", lambda e, ci=ci: e.tensor_copy(out=gT[0:8, ci * 512:(ci + 1) * 512], in_=psum[0:8, 4 + ci, 0:512]),
                 reads=[tP[4 + ci]], writes=[t_gT])
        barrier()
        moe_g, moe_u, moe_d = dr["moe_w_gate"], dr["moe_w_up"], dr["moe_w_down"]
        ov = outT.rearrange("(k p) n -> p k n", p=128)
        t_out = T("out")
        out_sv = []

        def final_chunk(ci, c0, w):
            bk = 1 + ci % 2
            S.op("act", lambda e: e.activation(out=sqb[:, :, 0:w], in_=xT[:, :, c0:c0 + w], func=AF.Square), reads=t_x[ci], writes=[t_sqb])
            mm_group(psum[:, bk, 0:w], [(onesD[:], sqb[:, k, 0:w]) for k in range(KC)], [t_ones, t_sqb], [tP[bk]])
            S.op("act", lambda e: e.activation(out=rstd[:, 0:w], in_=psum[:, bk, 0:w], func=AF.Ln, bias=epsc[:, 0:1]),
                 reads=[tP[bk], t_epsc], writes=[t_rstd])
            S.op("act", lambda e: e.activation(out=rstd[:, 0:w], in_=rstd[:, 0:w], func=AF.Exp, scale=-0.5), reads=[t_rstd], writes=[t_rstd])
            for k in range(KC):
                S.op("dve", lambda e, k=k: e.scalar_tensor_tensor(out=xs[:, k, 0:w], in0=xT[:, k, c0:c0 + w], scalar=small["gfinT"][:, k:k + 1],
                                                                 in1=rstd[:, 0:w], op0=ALU.mult, op1=ALU.mult),
                     reads=t_x[ci] + [tsm["gfinT"], t_rstd], writes=[t_xs])
            out_sv.append(S.dma("sp", lambda e: e.dma_start(out=ov[:, :, c0:c0 + w], in_=xs[:, :, 0:w]), reads=[t_xs], sem_t=t_out))
        for ex_i in range(NE):
            for ci, (c0, w) in enumerate(CHO):
                bk = 2 + (ci % 2)
                S.op("pe", lambda e, ex_i=ex_i, bk=bk, c0=c0, w=w: e.matmul(psum[:, bk, 0:w], lhsT=small["sel"][:, ex_i, :], rhs=gT[0:8, c0:c0 + w],
                                                                        start=True, stop=True), reads=[tsm["sel"], t_gT], writes=[tP[bk]])
                S.op("act", lambda e, ci=ci, bk=bk, w=w: e.activation(out=Gb[:, ci, 0:w], in_=psum[:, bk, 0:w], func=AF.Copy),
                     reads=[tP[bk]], writes=[t_Gb[ci]])
            ffn(moe_g[ex_i], moe_u[ex_i], moe_d[ex_i], FFE, [(ci, c0, w) for ci, (c0, w) in enumerate(CHO)],
                lambda k, c0, w: hT[:, k, c0:c0 + w], lambda ci: [t_h[ci]], lambda k, c0, w: hT[:, k, c0:c0 + w], lambda ci: [t_h[ci]],
                lambda d: modv[:, 1, 40 + d:41 + d], [t_mod], tok_gate=(lambda ci, w: Gb[:, ci, 0:w], lambda ci: [t_Gb[ci]]),
                on_chunk_done=(final_chunk if ex_i == NE - 1 else None), first_gu_preloaded=(ex_i == 0))
        assert len(out_sv) == 4
        S.final_waits.append(out_sv[-1])
        S.emit()
        return nc


def finish(nc, S, st, outT, xT, t_x, final=None):
    ov = outT.rearrange("(k p) n -> p k n", p=128)
    t_out = T("out")
    sv = None
    if final is None:
        for ci, (c0, w) in enumerate(CHO):
            sv = S.dma("sp", lambda e, c0=c0, w=w: e.dma_start(out=ov[:, :, c0:c0 + w], in_=xT[:, :, c0:c0 + w]), reads=t_x[ci], sem_t=t_out)
    else:
        (xs, t_xs, sqb, t_sqb, rstd, t_rstd, tmpA, t_tmpA, tmpB, t_tmpB, onesD, t_ones, small, tsm, psum, tP, epsc, t_epsc) = final
        for ci, (c0, w) in enumerate(CHO):
            bk = 1 + ci % 2
            for k in range(KC):
                S.op("act", lambda e, k=k, c0=c0, w=w: e.activation(out=sqb[:, k, 0:w], in_=xT[:, k, c0:c0 + w], func=AF.Square),
                     reads=t_x[ci], writes=[t_sqb])

            def fn(e, bk=bk, w=w):
                ins = None
                for k in range(KC):
                    ins = e.matmul(psum[:, bk, 0:w], lhsT=onesD[:], rhs=sqb[:, k, 0:w], start=(k == 0), stop=(k == KC - 1))
                return ins
            S.op("pe", fn, reads=[t_ones, t_sqb], writes=[tP[bk]])
            S.op("act", lambda e, bk=bk, w=w: e.activation(out=rstd[:, 0:w], in_=psum[:, bk, 0:w], func=AF.Sqrt, bias=epsc[:, 0:1]), reads=[tP[bk], t_epsc], writes=[t_rstd])
            S.op("dve", lambda e, w=w: e.reciprocal(out=rstd[:, 0:w], in_=rstd[:, 0:w]), reads=[t_rstd], writes=[t_rstd])
            for k in range(KC):
                S.op("dve", lambda e, k=k, c0=c0, w=w: e.scalar_tensor_tensor(out=xs[:, k, 0:w], in0=xT[:, k, c0:c0 + w], scalar=small["gfinT"][:, k:k + 1],
                                                                             in1=rstd[:, 0:w], op0=ALU.mult, op1=ALU.mult),
                     reads=t_x[ci] + [tsm["gfinT"], t_rstd], writes=[t_xs])
            sv = S.dma("sp", lambda e, c0=c0, w=w: e.dma_start(out=ov[:, :, c0:c0 + w], in_=xs[:, :, 0:w]), reads=[t_xs], sem_t=t_out)
    S.final_waits.append(sv)
    S.emit()
    return nc


def _consts():
    bf = ml_dtypes.bfloat16
    c = {}
    c["ident_b"] = np.eye(128, dtype=np.float32).astype(bf)
    c["ident_f"] = np.eye(128, dtype=np.float32)
    kk = np.arange(128)[:, None]
    qq = np.arange(512)[None, :]
    c["masks"] = np.stack([np.where(t * 128 + kk > qq, NEG, 0.0) for t in range(4)], axis=1).astype(np.float32).astype(bf)
    c["maskh"] = np.where(kk > 96 + np.arange(HALO)[None, :], NEG, 0.0).astype(np.float32).astype(bf)
    half = 32
    inv = (np.float32(10000.0) ** (-np.arange(half, dtype=np.float32) / np.float32(half))).astype(np.float32)
    c["invf"] = np.tile(inv, 4).reshape(128, 1).astype(np.float32)
    sel = np.zeros((8, NE, 128), np.float32)
    for e in range(NE):
        sel[e, e, :] = 1.0
    c["sel"] = sel
    return c


def _colT(v, n):
    return np.ascontiguousarray(np.asarray(v, np.float32).reshape(n, 128).T)


def make_in_maps(inp):
    cst = _consts()
    x = np.asarray(inp["x"], np.float32)
    pos = np.asarray(inp["positions"], np.int32)
    shared = {n: np.ascontiguousarray(np.asarray(inp[n], np.float32).reshape(sh)) for n, sh in W_SPECS}
    sm = dict(cst)
    sm["badaT"] = np.ascontiguousarray(np.asarray(inp["b_ada"], np.float32).reshape(2, 48, 128).transpose(2, 0, 1))
    sm["gmixT"] = np.ascontiguousarray(np.asarray(inp["norm_mix_g"], np.float32).reshape(2, KC, 128).transpose(2, 0, 1))
    sm["gffnT"] = np.ascontiguousarray(np.asarray(inp["norm_ffn_g"], np.float32).reshape(2, KC, 128).transpose(2, 0, 1))
    sm["gfinT"] = _colT(inp["final_g"], KC)
    lam = np.stack([np.asarray(inp[k], np.float32)[0] for k in ("lam_q1", "lam_k1", "lam_q2", "lam_k2")], 0)
    sm["lamv"] = np.ascontiguousarray(np.broadcast_to(lam[None], (128, 4, 64)))
    sm["gsub"] = np.ascontiguousarray(np.asarray(inp["attn_subln_g"], np.float32)[0].reshape(128, 1))
    sm["bpw1T"] = _colT(np.asarray(inp["conv_b_pw1"])[0], 16)
    sm["wdwT"] = np.ascontiguousarray(np.asarray(inp["conv_w_dw"], np.float32)[0].reshape(31, KC, 128).transpose(2, 1, 0))
    sm["bdwT"] = _colT(np.asarray(inp["conv_b_dw"])[0], KC)
    sm["lngT"] = _colT(np.asarray(inp["conv_ln_g"])[0], KC)
    sm["lnbT"] = _colT(np.asarray(inp["conv_ln_b"])[0], KC)
    sm["bpw2T"] = _colT(np.asarray(inp["conv_b_pw2"])[0], KC)
    maps = []
    for core in range(8):
        b, h = core // 2, core % 2
        m = dict(shared)
        m.update(sm)
        own = x[b, h * NO:(h + 1) * NO, :]
        if h == 1:
            prev = x[b, 0:NP, :]
            halo = prev[NP - HALO:, :]
            ppos = pos[b, 0:NP]
        else:
            prev = np.zeros((NP, D), np.float32)
            halo = np.zeros((HALO, D), np.float32)
            ppos = np.zeros((NP,), np.int32)
        m["xT_all"] = np.ascontiguousarray(np.concatenate([own, halo], 0).T)
        m["xT_prev"] = np.ascontiguousarray(prev.T)
        m["pos_kv"] = np.ascontiguousarray(np.concatenate([ppos, pos[b, h * NO:(h + 1) * NO]])[None, :].astype(np.int32))
        m["cT"] = _colT(np.asarray(inp["c"], np.float32)[b], KC)
        m["visb"] = np.full((128, 1), 0.0 if h == 1 else NEG, np.float32)
        m["hflag"] = np.full((128, 1), 1.0 if h == 1 else 0.0, np.float32)
        maps.append(m)
    return maps


_NC_CACHE = {}


def run(inp, stage=99):
    if stage not in _NC_CACHE:
        _NC_CACHE[stage] = build_program(stage)
    nc = _NC_CACHE[stage]
    maps = make_in_maps(inp)
    res = run_bass_kernel_spmd(nc, maps, core_ids=list(range(8)))
    out = np.empty((4, 2 * NO, D), np.float32)
    for core in range(8):
        b, h = core // 2, core % 2
        out[b, h * NO:(h + 1) * NO, :] = res.results[core]["outT"].T
    return out


def kernel(**inputs):
    return run(inputs, 99)
```
